# Optimizing a Trainium2 kernel written in Bass

```python
import math
import jax, jax.numpy as jnp
from jax import lax
import numpy as np

D_MODEL = 1024
BATCH = 8
SEQ = 4096
DEPTH = 2

GRID_W = 64
CTX_LEN = 256
HEAD_DIM = 64
ROPE_BASE = 10000.0
RMS_EPS = 1e-6

GLA_HEADS = 4
GLA_DK = 32
GLA_DV = 64
GLA_LR = 16
GLA_NORMALIZER = 16.0
GLA_CHUNK = 64
DIFF_HEADS = 4
DIFF_HD = 64
DIFF_QB = 128
NA_HEADS = 4
NA_HD = 64
WIN_R = 8
WIN_C = 16

GLA_QK = GLA_HEADS * GLA_DK
GLA_V = GLA_HEADS * GLA_DV
DIFF_QK = DIFF_HEADS * 2 * DIFF_HD
DIFF_V = DIFF_HEADS * 2 * DIFF_HD
NA_W = NA_HEADS * NA_HD
MIX_WIDTH = GLA_V + DIFF_V + NA_W
IN_NAMES = ('gq', 'gk', 'gv', 'gg', 'gdf', 'gdb', 'dq', 'dk', 'dv', 'nq', 'nk', 'nv')
IN_SIZES = (GLA_QK, GLA_QK, GLA_V, GLA_V, GLA_LR, GLA_LR, DIFF_QK, DIFF_QK, DIFF_V, NA_W, NA_W, NA_W)
IN_COLS = sum(IN_SIZES)

FFN_DENSE = 2816
N_EXPERTS = 8
TOP_K = 2
FFN_EXPERT = 3584
N_DENSE = (DEPTH + 1) // 2
N_MOE = DEPTH // 2

kernel_name = 'hymba_gla_diff_natten_moe_prefix_dit'


def rmsnorm(x, g):
    xf = x.astype(jnp.float32)
    y = xf * lax.rsqrt(jnp.mean(xf * xf, axis=-1, keepdims=True) + RMS_EPS)
    return (y * g.astype(jnp.float32)).astype(x.dtype)


def modulate(h, shift, scale):
    return h * (1.0 + scale) + shift


def project(h, w_in):
    z = jnp.einsum('bld,de->ble', h, w_in)
    offs = [int(o) for o in np.cumsum(IN_SIZES)[:-1]]
    return dict(zip(IN_NAMES, jnp.split(z, offs, axis=-1)))


def axial_rope_angles(n_tokens):
    t = jnp.arange(n_tokens)
    row = (t // GRID_W).astype(jnp.float32)
    col = (t % GRID_W).astype(jnp.float32)
    quarter = HEAD_DIM // 4
    inv = 1.0 / (ROPE_BASE ** (jnp.arange(quarter, dtype=jnp.float32) / quarter))
    return row[:, None] * inv, col[:, None] * inv


def rope_rotate(x, ang):
    x1, x2 = jnp.split(x, 2, axis=-1)
    cos, sin = jnp.cos(ang), jnp.sin(ang)
    return jnp.concatenate([x1 * cos - x2 * sin, x1 * sin + x2 * cos], axis=-1)


def apply_axial_rope(x, ang_r, ang_c):
    xf = x.astype(jnp.float32)
    half = HEAD_DIM // 2
    ar = ang_r[:, None, None, :]
    ac = ang_c[:, None, None, :]
    out = jnp.concatenate([rope_rotate(xf[..., :half], ar), rope_rotate(xf[..., half:], ac)], axis=-1)
    return out.astype(x.dtype)


def gla_log_decay(zd, w_up, b):
    logits = jnp.einsum('blr,rk->blk', zd, w_up) + b
    return jax.nn.log_sigmoid(logits.astype(jnp.float32)) / GLA_NORMALIZER


def gla_inputs(p, w_dec_up, b_dec):
    B, L, _ = p['gq'].shape
    heads = lambda t, d: t.reshape(B, L, GLA_HEADS, d)
    q = heads(p['gq'], GLA_DK) * GLA_DK ** -0.5
    k = heads(p['gk'], GLA_DK)
    v = heads(p['gv'], GLA_DV)
    lg_f = heads(gla_log_decay(p['gdf'], w_dec_up[0], b_dec[0]), GLA_DK)
    lg_b = heads(gla_log_decay(p['gdb'], w_dec_up[1], b_dec[1]), GLA_DK)
    return q, k, v, lg_f, lg_b


def gla_chunked(q, k, v, log_g, s0, with_output):
    B, L, H, _ = q.shape
    dv = v.shape[-1]
    n = L // GLA_CHUNK
    blk = lambda t: t.astype(jnp.float32).reshape(B, n, GLA_CHUNK, H, t.shape[-1])
    q, k, v, log_g = blk(q), blk(k), blk(v), blk(log_g)
    b = jnp.cumsum(log_g, axis=2)
    b_end = b[:, :, -1]
    k_end = k * jnp.exp(b_end[:, :, None] - b)
    u = jnp.einsum('bnshk,bnshv->bnhkv', k_end, v)

    def step(s, inp):
        lb, u_c = inp
        return jnp.exp(lb)[..., None] * s + u_c, s

    s_fin, s_prev = lax.scan(step, s0, (jnp.moveaxis(b_end, 1, 0), jnp.moveaxis(u, 1, 0)))
    if not with_output:
        return None, s_fin
    q_dec = q * jnp.exp(b)
    k_inv = k * jnp.exp(-b)
    lower = jnp.tril(jnp.ones((GLA_CHUNK, GLA_CHUNK), dtype=bool))
    att = jnp.where(lower, jnp.einsum('bnchk,bnshk->bnhcs', q_dec, k_inv), 0.0)
    o = (jnp.einsum('bnhcs,bnshv->bnchv', att, v)
         + jnp.einsum('bnchk,nbhkv->bnchv', q_dec, s_prev))
    return o.reshape(B, L, H, dv), s_fin


def gla_bidir(q, k, v, lg_f, lg_b, s0_f, s0_b, with_output):
    flip = lambda t: jnp.flip(t, axis=1)
    o_f, s_f = gla_chunked(q, k, v, lg_f, s0_f, with_output)
    o_b, s_b = gla_chunked(flip(q), flip(k), flip(v), flip(lg_b), s0_b, with_output)
    o = o_f + flip(o_b) if with_output else None
    return o, s_f, s_b


def gla_out(o, gate, g):
    B, L = o.shape[:2]
    o = rmsnorm(o, g).reshape(B, L, GLA_V).astype(gate.dtype)
    return o * jax.nn.silu(gate)


def diff_lambda_value(lam, layer_idx):
    lam_init = 0.8 - 0.6 * math.exp(-0.3 * layer_idx)
    lf = lam.astype(jnp.float32)
    lam_full = jnp.exp(jnp.sum(lf[0] * lf[1])) - jnp.exp(jnp.sum(lf[2] * lf[3])) + lam_init
    return lam_full, lam_init


def diff_attend(q, k, v, lam):
    s = jnp.einsum('bhmqd,bhmkd->bhmqk', q * DIFF_HD ** -0.5, k).astype(jnp.float32)
    p = jax.nn.softmax(s, axis=-1)
    a = p[:, :, 0] - lam * p[:, :, 1]
    return jnp.einsum('bhqk,bhkv->bhqv', a.astype(v.dtype), v)


def diff_blocked(q, k, v, lam):
    B, H, M, L, d = q.shape
    nb = L // DIFF_QB
    qb = jnp.moveaxis(q.reshape(B, H, M, nb, DIFF_QB, d), 3, 0)
    ob = lax.map(lambda qq: diff_attend(qq, k, v, lam), qb)
    return jnp.moveaxis(ob, 0, 2).reshape(B, H, L, v.shape[-1])


def diff_out(o, g, lam_init):
    o = rmsnorm(o, g) * (1.0 - lam_init)
    B, H, L, dv = o.shape
    return o.transpose(0, 2, 1, 3).reshape(B, L, H * dv)


def neighbourhood_attention(q, k, v, k_ctx, v_ctx, rpb):
    B, S, H, d = q.shape
    rows = S // GRID_W
    wr = min(WIN_R, rows)
    grid = lambda t: t.reshape(B, rows, GRID_W, H, d)
    qg, kg, vg = grid(q), grid(k), grid(v)
    cols = jnp.arange(GRID_W)
    cstart = jnp.clip(cols - WIN_C // 2, 0, GRID_W - WIN_C)
    cidx = cstart[:, None] + jnp.arange(WIN_C)[None, :]
    cbias = cidx - cols[:, None] + WIN_C - 1
    nloc = wr * WIN_C
    scale = d ** -0.5

    def row_fn(r):
        rs = jnp.clip(r - WIN_R // 2, 0, rows - wr)
        kw = lax.dynamic_slice_in_dim(kg, rs, wr, axis=1)[:, :, cidx]
        vw = lax.dynamic_slice_in_dim(vg, rs, wr, axis=1)[:, :, cidx]
        qr = lax.dynamic_index_in_dim(qg, r, axis=1, keepdims=False) * scale
        rbias = rs + jnp.arange(wr) - r + WIN_R - 1
        bias = rpb[:, rbias[None, :, None], cbias[:, None, :]]
        s_loc = jnp.einsum('bchd,brcjhd->bhcrj', qr, kw).astype(jnp.float32) + bias.astype(jnp.float32)
        s_ctx = jnp.einsum('bchd,bkhd->bhck', qr, k_ctx).astype(jnp.float32)
        s = jnp.concatenate([s_loc.reshape(B, H, GRID_W, nloc), s_ctx], axis=-1)
        p = jax.nn.softmax(s, axis=-1).astype(v.dtype)
        p_loc = p[..., :nloc].reshape(B, H, GRID_W, wr, WIN_C)
        p_ctx = p[..., nloc:]
        return (jnp.einsum('bhcrj,brcjhd->bchd', p_loc, vw)
                + jnp.einsum('bhck,bkhd->bchd', p_ctx, v_ctx))

    out = lax.map(row_fn, jnp.arange(rows))
    return jnp.moveaxis(out, 0, 1).reshape(B, S, H * d)


def softmax_attend(q, k, v):
    s = jnp.einsum('bqhd,bkhd->bhqk', q * q.shape[-1] ** -0.5, k).astype(jnp.float32)
    p = jax.nn.softmax(s, axis=-1).astype(v.dtype)
    return jnp.einsum('bhqk,bkhd->bqhd', p, v)


def mixer(h_lat, h_ctx, w_in, w_dec_up, b_dec, g_gla, lam, g_diff, rpb, w_out,
          layer_idx, ang_r, ang_c, with_ctx_out):
    B, S, _ = h_lat.shape
    C = h_ctx.shape[1]
    pl, pc = project(h_lat, w_in), project(h_ctx, w_in)

    ql, kl, vl, lfl, lbl = gla_inputs(pl, w_dec_up, b_dec)
    qc, kc, vc, lfc, lbc = gla_inputs(pc, w_dec_up, b_dec)
    zero = jnp.zeros((B, GLA_HEADS, GLA_DK, GLA_DV), jnp.float32)
    oc_gla, s_f, s_b = gla_bidir(qc, kc, vc, lfc, lbc, zero, zero, with_ctx_out)
    ol_gla, _, _ = gla_bidir(ql, kl, vl, lfl, lbl, s_f, s_b, True)
    a_lat = gla_out(ol_gla, pl['gg'], g_gla)

    lam_full, lam_init = diff_lambda_value(lam, layer_idx)

    def diff_qkv(p, L):
        return (p['dq'].reshape(B, L, DIFF_HEADS, 2, DIFF_HD),
                p['dk'].reshape(B, L, DIFF_HEADS, 2, DIFF_HD),
                p['dv'].reshape(B, L, DIFF_HEADS, 2 * DIFF_HD))

    dql, dkl, dvl = diff_qkv(pl, S)
    dql = apply_axial_rope(dql, ang_r, ang_c)
    dkl = apply_axial_rope(dkl, ang_r, ang_c)
    dqc, dkc, dvc = diff_qkv(pc, C)
    maps_first = lambda t: t.transpose(0, 2, 3, 1, 4)
    heads_first = lambda t: t.transpose(0, 2, 1, 3)
    k_all = jnp.concatenate([maps_first(dkl), maps_first(dkc)], axis=3)
    v_all = jnp.concatenate([heads_first(dvl), heads_first(dvc)], axis=2)
    b_lat = diff_out(diff_blocked(maps_first(dql), k_all, v_all, lam_full), g_diff, lam_init)

    na = lambda p, name, L: p[name].reshape(B, L, NA_HEADS, NA_HD)
    nkc, nvc = na(pc, 'nk', C), na(pc, 'nv', C)
    c_lat = neighbourhood_attention(na(pl, 'nq', S), na(pl, 'nk', S), na(pl, 'nv', S), nkc, nvc, rpb)

    y_lat = jnp.concatenate([a_lat, b_lat, c_lat], axis=-1) @ w_out
    if not with_ctx_out:
        return y_lat, None
    a_ctx = gla_out(oc_gla, pc['gg'], g_gla)
    b_ctx = diff_out(diff_attend(maps_first(dqc), maps_first(dkc), heads_first(dvc), lam_full), g_diff, lam_init)
    c_ctx = softmax_attend(na(pc, 'nq', C), nkc, nvc).reshape(B, C, NA_W)
    y_ctx = jnp.concatenate([a_ctx, b_ctx, c_ctx], axis=-1) @ w_out
    return y_lat, y_ctx


def swiglu(h, wg, wu, wd):
    return (jax.nn.silu(h @ wg) * (h @ wu)) @ wd


def moe_swiglu(h, w_router, wg, wu, wd):
    shp = h.shape
    t = h.reshape(-1, shp[-1])
    logits = (t @ w_router).astype(jnp.float32)
    top_v, top_i = lax.top_k(logits, TOP_K)
    top_w = jax.nn.softmax(top_v, axis=-1)
    gates = jnp.sum(jax.nn.one_hot(top_i, N_EXPERTS, dtype=jnp.float32) * top_w[..., None], axis=1)
    y = jnp.zeros_like(t)
    for e in range(N_EXPERTS):
        y = y + gates[:, e:e + 1].astype(t.dtype) * swiglu(t, wg[e], wu[e], wd[e])
    return y.reshape(shp)


def setup_inputs(seed: int = 0) -> dict:
    key = jax.random.key(seed)
    ks = jax.random.split(key, 24)
    f32 = jnp.float32
    nrm = lambda k, shape, s: jax.random.normal(k, shape, f32) * s
    D = D_MODEL
    return {
        'x': nrm(ks[0], (BATCH, SEQ, D), 1.0),
        'c': nrm(ks[1], (BATCH, D), 1.0),
        'ctx': nrm(ks[2], (BATCH, CTX_LEN, D), 1.0),
        'c_ctx': nrm(ks[3], (D,), 1.0),
        'w_mod': nrm(ks[4], (DEPTH, D, 6 * D), 0.5 * D ** -0.5),
        'b_mod': nrm(ks[5], (DEPTH, 6 * D), 0.01),
        'g_norm1': 1.0 + nrm(ks[6], (DEPTH, D), 0.1),
        'g_norm2': 1.0 + nrm(ks[7], (DEPTH, D), 0.1),
        'w_in': nrm(ks[8], (DEPTH, D, IN_COLS), D ** -0.5),
        'gla_w_dec_up': nrm(ks[9], (DEPTH, 2, GLA_LR, GLA_QK), GLA_LR ** -0.5),
        'gla_b_dec': nrm(ks[10], (DEPTH, 2, GLA_QK), 0.1),
        'gla_g_norm': 1.0 + nrm(ks[11], (DEPTH, GLA_DV), 0.1),
        'diff_lambda': nrm(ks[12], (DEPTH, 4, DIFF_HD), 0.1),
        'diff_g_norm': 1.0 + nrm(ks[13], (DEPTH, 2 * DIFF_HD), 0.1),
        'na_rpb': nrm(ks[14], (DEPTH, NA_HEADS, 2 * WIN_R - 1, 2 * WIN_C - 1), 0.1),
        'w_out': nrm(ks[15], (DEPTH, MIX_WIDTH, D), MIX_WIDTH ** -0.5),
        'w_ffn_gate': nrm(ks[16], (N_DENSE, D, FFN_DENSE), D ** -0.5),
        'w_ffn_up': nrm(ks[17], (N_DENSE, D, FFN_DENSE), D ** -0.5),
        'w_ffn_down': nrm(ks[18], (N_DENSE, FFN_DENSE, D), FFN_DENSE ** -0.5),
        'w_router': nrm(ks[19], (N_MOE, D, N_EXPERTS), D ** -0.5),
        'w_moe_gate': nrm(ks[20], (N_MOE, N_EXPERTS, D, FFN_EXPERT), D ** -0.5),
        'w_moe_up': nrm(ks[21], (N_MOE, N_EXPERTS, D, FFN_EXPERT), D ** -0.5),
        'w_moe_down': nrm(ks[22], (N_MOE, N_EXPERTS, FFN_EXPERT, D), FFN_EXPERT ** -0.5),
        'g_final': 1.0 + nrm(ks[23], (D,), 0.1),
    }


def reference(x, c, ctx, c_ctx, w_mod, b_mod, g_norm1, g_norm2, w_in, gla_w_dec_up, gla_b_dec,
              gla_g_norm, diff_lambda, diff_g_norm, na_rpb, w_out, w_ffn_gate, w_ffn_up, w_ffn_down,
              w_router, w_moe_gate, w_moe_up, w_moe_down, g_final):
    S = x.shape[1]
    ang_r, ang_c = axial_rope_angles(S)
    ctx_s = ctx
    for l in range(DEPTH):
        last = l == DEPTH - 1
        sh1, sc1, ga1, sh2, sc2, ga2 = jnp.split(jax.nn.silu(c) @ w_mod[l] + b_mod[l], 6, axis=-1)
        csh1, csc1, cga1, csh2, csc2, cga2 = jnp.split(jax.nn.silu(c_ctx) @ w_mod[l] + b_mod[l], 6, axis=-1)

        h_lat = modulate(rmsnorm(x, g_norm1[l]), sh1[:, None], sc1[:, None])
        h_ctx = modulate(rmsnorm(ctx_s, g_norm1[l]), csh1, csc1)
        y_lat, y_ctx = mixer(h_lat, h_ctx, w_in[l], gla_w_dec_up[l], gla_b_dec[l], gla_g_norm[l],
                             diff_lambda[l], diff_g_norm[l], na_rpb[l], w_out[l],
                             l, ang_r, ang_c, not last)
        x = x + ga1[:, None] * y_lat
        if not last:
            ctx_s = ctx_s + cga1 * y_ctx

        if l % 2 == 0:
            i = l // 2
            ffn = lambda h, i=i: swiglu(h, w_ffn_gate[i], w_ffn_up[i], w_ffn_down[i])
        else:
            i = l // 2
            ffn = lambda h, i=i: moe_swiglu(h, w_router[i], w_moe_gate[i], w_moe_up[i], w_moe_down[i])
        h_lat = modulate(rmsnorm(x, g_norm2[l]), sh2[:, None], sc2[:, None])
        x = x + ga2[:, None] * ffn(h_lat)
        if not last:
            h_ctx = modulate(rmsnorm(ctx_s, g_norm2[l]), csh2, csc2)
            ctx_s = ctx_s + cga2 * ffn(h_ctx)
    return rmsnorm(x, g_final)
```

```python
import contextlib, math
import numpy as np
import ml_dtypes
import concourse.bass as bass
import concourse.mybir as mybir
from concourse.bass_utils import run_bass_kernel_spmd

F32 = mybir.dt.float32
BF16 = mybir.dt.bfloat16
AF = mybir.ActivationFunctionType
ALU = mybir.AluOpType
AX = mybir.AxisListType

L = 4096; C = 256; T = 4352; D = 1024; NT = 34; DEPTH = 2
INC = 3104
FD = 2816; FE = 3584; NE = 8
EPS = 1e-6
NEG = -30000.0

SAME_ENG_SYNC = True
BANK = 30000
NDMA = 8


class Sched:
    ENGS = ['pe', 'dve', 'act', 'pool', 'sp']

    def __init__(self, nc, es):
        self.nc = nc
        self.es = es
        self.ops = []
        self.lastw = {}
        self.readers = {}
        self.dmahist = {e: [] for e in self.ENGS}
        self.last_on = {e: None for e in self.ENGS}
        self.nown = 0
        self.casthist = []

    def _deps(self, reads, writes):
        deps = set()
        for k in reads:
            w = self.lastw.get(k)
            if w is not None:
                deps.add(w)
        for k in writes:
            w = self.lastw.get(k)
            if w is not None:
                deps.add(w)
            for r in self.readers.get(k, ()):
                deps.add(r)
        return deps

    def _prune(self, deps):
        best = {}
        out = set()
        for d in deps:
            od = self.ops[d]
            if od['kind'] == 'op':
                e = od['eng']
                if e not in best or d > best[e]:
                    best[e] = d
            else:
                out.add(d)
        out.update(best.values())
        return out

    def _commit(self, idx, reads, writes):
        for k in reads:
            self.readers.setdefault(k, []).append(idx)
        for k in writes:
            self.lastw[k] = idx
            self.readers[k] = []

    def op(self, eng, fn, reads=(), writes=()):
        deps = self._prune(self._deps(reads, writes))
        idx = len(self.ops)
        self.ops.append(dict(eng=eng, fn=fn, deps=deps, kind='op', inc=False))
        self._commit(idx, reads, writes)
        self.last_on[eng] = idx
        return idx

    def dma(self, q, out, in_, reads=(), writes=(), own=False):
        deps = self._prune(self._deps(reads, writes))
        idx = len(self.ops)
        o = dict(eng=q, fn=(lambda e, o=out, i=in_: e.dma_start(out=o, in_=i)),
                 deps=deps, kind='dma', inc=True)
        if own:
            h = self.casthist
            if len(h) >= 2:
                deps.add(h[-2])
            o['cn'] = len(h)
            h.append(idx)
        else:
            h = self.dmahist[q]
            if len(h) >= NDMA:
                deps.add(h[-NDMA])
            o['n'] = len(h)
            h.append(idx)
        self.ops.append(o)
        self._commit(idx, reads, writes)
        return idx

    def _alltoks(self):
        toks = set()
        for e in self.ENGS:
            if self.last_on[e] is not None:
                toks.add(self.last_on[e])
            for d in self.dmahist[e][-NDMA:]:
                toks.add(d)
        return toks

    def barrier(self):
        toks = self._alltoks()
        for e in self.ENGS:
            self.ops.append(dict(eng=e, fn=None, deps=set(toks), kind='bar', inc=False))
        self.lastw = {k: v for k, v in self.lastw.items() if isinstance(k, str) and k.startswith('@')}
        self.readers = {k: v for k, v in self.readers.items() if isinstance(k, str) and k.startswith('@')}

    def emit(self, final_wait_eng='sp'):
        nc = self.nc
        ops = self.ops
        toks = self._alltoks()
        for i_ in self.casthist[-2:]:
            toks.add(i_)
        ops.append(dict(eng=final_wait_eng, fn=None, deps=toks, kind='bar', inc=False))
        for o in ops:
            for d in o['deps']:
                od = ops[d]
                if od['kind'] == 'op':
                    if od['eng'] == o['eng'] and (od['eng'] == 'pe' or not SAME_ENG_SYNC):
                        continue
                    od['inc'] = True
        cnt = {e: 0 for e in self.ENGS}
        for o in ops:
            if o['kind'] == 'op' and o['inc']:
                c = cnt[o['eng']]
                o['tok'] = (o['eng'], c // BANK, c % BANK + 1)
                cnt[o['eng']] = c + 1
            elif o['kind'] == 'dma':
                if 'cn' in o:
                    o['tok'] = ('cast', o['cn'] % 2, 16 * (o['cn'] // 2 + 1))
                else:
                    n = o['n']
                    o['tok'] = ('d' + o['eng'], n % NDMA, 16 * (n // NDMA + 1))
        sems = {}

        def getsem(name, bank):
            key = (name, bank)
            if key not in sems:
                sems[key] = self.es.enter_context(nc.semaphore(f"s_{name}_{bank}"))
            return sems[key]

        for e in self.ENGS:
            for b in range(cnt[e] // BANK + 1):
                getsem(e, b)
            if self.dmahist[e]:
                for s in range(NDMA):
                    getsem('d' + e, s)
        if self.casthist:
            getsem('cast', 0); getsem('cast', 1)
        blk = self.es.enter_context(nc.Block())
        nwaits = {}
        self.nwaits = nwaits

        def run(ename):
            def body(e):
                seen = {}
                for o in ops:
                    if o['eng'] != ename:
                        continue
                    for d in sorted(o['deps']):
                        od = ops[d]
                        if 'tok' not in od:
                            continue
                        if od['kind'] == 'op' and od['eng'] == ename and (ename == 'pe' or not SAME_ENG_SYNC):
                            continue
                        name, bank, val = od['tok']
                        if seen.get((name, bank), 0) >= val:
                            continue
                        e.wait_ge(getsem(name, bank), val)
                        nwaits[ename] = nwaits.get(ename, 0) + 1
                        seen[(name, bank)] = val
                    if o['fn'] is None:
                        continue
                    inst = o['fn'](e)
                    if o['kind'] == 'dma':
                        name, bank, val = o['tok']
                        inst.then_inc(getsem(name, bank), 16)
                    elif o['inc']:
                        name, bank, val = o['tok']
                        inst.then_inc(getsem(name, bank), 1)
            return body

        blk.tensor(run('pe'))
        blk.vector(run('dve'))
        blk.scalar(run('act'))
        blk.gpsimd(run('pool'))
        blk.sync(run('sp'))
        self.stats = dict(nops=len(ops), cnt=cnt, nsem=len(sems))


class Arena:
    def __init__(self, nc, es, nwords):
        self.t = es.enter_context(nc.sbuf_tensor("arena", [128, nwords], F32))
        self.n = nwords
        self.off = 0

    def mark(self):
        return self.off

    def reset(self, m):
        self.off = m

    def alloc(self, shape, dtype, parts=128):
        n = int(np.prod(shape))
        words = n if dtype == F32 else (n + 1) // 2
        a = self.t[0:parts, self.off:self.off + words]
        self.off += words
        assert self.off <= self.n, f"arena overflow {self.off} > {self.n}"
        if dtype == BF16:
            a = a.bitcast(BF16)
            if 2 * words != n:
                a = a[:, 0:n]
        if len(shape) == 2:
            a = a.rearrange("p (a b) -> p a b", a=shape[0], b=shape[1])
        elif len(shape) == 3:
            a = a.rearrange("p (a b c) -> p a b c", a=shape[0], b=shape[1], c=shape[2])
        return a


def _consts():
    c32 = {}
    cb = {}
    s = np.arange(128)[:, None]
    c = np.arange(128)[None, :]
    g = -1.0 / 16.0
    c32['Uf'] = np.where(s <= c, g, 0.0)
    c32['Ub'] = np.where(s >= c, g, 0.0)
    c32['UfX'] = np.where(s > c, g, 0.0)
    c32['UbX'] = np.where(s < c, g, 0.0)
    bm = np.zeros((128, 256)); mh = np.zeros((128, 4))
    for h in range(4):
        bm[h * 32:(h + 1) * 32, h * 64:(h + 1) * 64] = 1.0
        mh[h * 32:(h + 1) * 32, h] = 32.0 ** -0.5
    c32['bm'] = bm
    c32['maskh'] = mh
    c32['ident'] = np.eye(128)
    t = np.arange(L)
    row = (t // 64).astype(np.float32); col = (t % 64).astype(np.float32)
    inv = (1.0 / (np.float32(10000.0) ** (np.arange(16, dtype=np.float32) / np.float32(16)))).astype(np.float32)
    ang_r = row[:, None] * inv[None, :]
    ang_c = col[:, None] * inv[None, :]
    cos = np.zeros((64, L), np.float32); sin = np.zeros((64, L), np.float32)
    for d in range(64):
        a = ang_r if d < 32 else ang_c
        cos[d] = np.cos(a[:, d % 16]); sin[d] = np.sin(a[:, d % 16])
    rt = np.zeros((64, 64), np.float32)
    for m in range(64):
        if (m % 32) < 16:
            rt[m + 16, m] = -1.0
        else:
            rt[m - 16, m] = 1.0
    cb['ident'] = np.eye(128)
    cb['maskF'] = np.broadcast_to(np.where(s <= c, 1.0, 0.0)[:, None, :], (128, 4, 128)).reshape(128, 512)
    cb['maskB'] = np.broadcast_to(np.where(s >= c, 1.0, 0.0)[:, None, :], (128, 4, 128)).reshape(128, 512)
    rtp = np.zeros((128, 128)); rtp[:64, :64] = rt; rtp[64:, 64:] = rt
    cb['RT'] = rtp
    c32['ones'] = np.ones((128, 128))
    off32 = {}; o = 0
    for k, v in c32.items():
        off32[k] = (o, v.shape[1]); o += v.shape[1]
    a32 = np.concatenate([v for v in c32.values()], axis=1).astype(np.float32)
    offb = {}; o = 0
    for k, v in cb.items():
        offb[k] = (o, v.shape[1]); o += v.shape[1]
    ab = np.concatenate([v for v in cb.values()], axis=1).astype(ml_dtypes.bfloat16)
    cos = np.concatenate([cos, cos], axis=0); sin = np.concatenate([sin, sin], axis=0)
    return a32, off32, ab, offb, cos, sin


def _na_bias(rpb):
    qc = np.arange(64)
    cstart = np.clip(qc - 8, 0, 48)
    kc = np.arange(64)
    valid = (kc[:, None] >= cstart[None, :]) & (kc[:, None] < cstart[None, :] + 16)
    cidx = np.clip(kc[:, None] - qc[None, :] + 15, 0, 30)
    out = np.full((DEPTH, 4, 2, 64, 14, 64), NEG, np.float32)
    for d0i in range(14):
        d0 = d0i - 7
        for rr in range(2):
            dr = d0 + rr
            if dr < -7 or dr > 7:
                continue
            g = rpb[:, :, dr + 7, :][:, :, cidx]
            out[:, :, rr, :, d0i, :] = np.where(valid[None, None], g, np.float32(NEG))
    return out.reshape(DEPTH, 4, 128, 14, 64)


class K:
    def __init__(self, debug=False, layers=(0, 1), stop_after=None):
        self.debug = debug
        self.layers = layers
        self.stop_after = stop_after
        self.nc = bass.Bass("TRN2", target_bir_lowering=False)
        self.es = contextlib.ExitStack()
        self.S = Sched(self.nc, self.es)
        self.A = Arena(self.nc, self.es, 51500)
        self.ps = [self.es.enter_context(self.nc.psum_tensor(f"ps{i}", [128, 512], F32)) for i in range(8)]
        self.din = {}
        self.dsc = {}
        self.uid = 0

    def inp(self, name, shape, dtype=F32):
        self.din[name] = self.nc.dram_tensor(name, list(shape), dtype, kind="ExternalInput").ap()
        return self.din[name]

    def scratch(self, name, shape, dtype, dbg=True):
        kind = "ExternalOutput" if (self.debug and dbg) else "Internal"
        self.dsc[name] = self.nc.dram_tensor(name, list(shape), dtype, kind=kind).ap()
        return self.dsc[name]

    def mm(self, out, lhsT, rhs, start, stop, r, w):
        self.S.op('pe', lambda e: e.matmul(out, lhsT=lhsT, rhs=rhs, start=start, stop=stop), r, w)

    def tr(self, out, in_, ident, r, w):
        self.S.op('pe', lambda e: e.transpose(out=out, in_=in_, identity=ident), r, w)

    def act(self, out, in_, func, r, w, **kw):
        self.S.op('act', lambda e: e.activation(out=out, in_=in_, func=func, **kw), r, w)

    def tt(self, eng, out, in0, in1, op, r, w):
        self.S.op(eng, lambda e: e.tensor_tensor(out=out, in0=in0, in1=in1, op=op), r, w)

    def ts(self, eng, out, in0, s1, s2, op0, op1, r, w, **kw):
        if s2 is None:
            self.S.op(eng, lambda e: e.tensor_scalar(out=out, in0=in0, scalar1=s1, scalar2=None, op0=op0, **kw), r, w)
        else:
            self.S.op(eng, lambda e: e.tensor_scalar(out=out, in0=in0, scalar1=s1, scalar2=s2, op0=op0, op1=op1, **kw), r, w)

    def stt(self, eng, out, in0, scalar, in1, op0, op1, r, w, **kw):
        self.S.op(eng, lambda e: e.scalar_tensor_tensor(out=out, in0=in0, scalar=scalar, in1=in1, op0=op0, op1=op1, **kw), r, w)

    def cp(self, eng, out, in_, r, w):
        if eng == 'act':
            self.S.op('act', lambda e: e.activation(out=out, in_=in_, func=AF.Copy), r, w)
        else:
            self.S.op(eng, lambda e: e.tensor_copy(out=out, in_=in_), r, w)

    def red(self, eng, out, in_, op, r, w):
        self.S.op(eng, lambda e: e.tensor_reduce(out=out, in_=in_, axis=AX.X, op=op), r, w)

    def recip(self, out, in_, r, w):
        self.S.op('dve', lambda e: e.reciprocal(out=out, in_=in_), r, w)

    def memset(self, eng, ap, val, w):
        self.S.op(eng, lambda e: e.memset(ap, val), (), w)

    def dma(self, q, out, in_, r=(), w=(), own=False):
        self.S.dma(q, out, in_, r, w, own=own)
        if q == 'pool' and not own:
            self.npool = getattr(self, 'npool', 0) + 1
            if self.npool % 4 == 0:
                self.issue_cast()

    def issue_cast(self):
        pc = getattr(self, 'pending_casts', [])
        if not pc:
            return
        d_, s_, key = pc.pop(0)
        k = getattr(self, 'ncast', 0)
        self.ncast = k + 1
        self.S.dma('pool', d_, s_, (), [key], own=True)

    def flush_casts(self, prefix):
        while getattr(self, 'pending_casts', []) and self.pending_casts[0][2].startswith(prefix):
            self.issue_cast()

    def rstd(self, ss, n, key):
        self.ts('dve', ss, ss, 1.0 / n, EPS, ALU.mult, ALU.add, [key], [key])
        self.act(ss, ss, AF.Sqrt, [key], [key])
        self.recip(ss, ss, [key], [key])

    def setup(self):
        nc = self.nc
        a32, off32, ab, offb, cos, sin = _consts()
        self.host_consts = dict(c32=a32, cb=ab, cos=cos, sin=sin)
        inp = self.inp
        inp('xin', [T, D]); inp('cc', [128, 8, 2])
        inp('w_mod', [DEPTH, D, 6 * D]); inp('b_mod', [DEPTH, 6 * D])
        inp('g_norm1', [DEPTH, D]); inp('g_norm2', [DEPTH, D]); inp('g_final', [D])
        inp('w_in', [DEPTH, D, INC]); inp('wup', [DEPTH, 2, 33, 128])
        inp('gla_g', [DEPTH, 256]); inp('diff_lambda', [DEPTH, 256]); inp('diff_g', [DEPTH, 128])
        inp('nabias', [DEPTH, 4, 128, 14, 64]); inp('w_out', [DEPTH, D, D])
        inp('w_ffn_gate', [1, D, FD]); inp('w_ffn_up', [1, D, FD]); inp('w_ffn_down', [1, FD, D])
        inp('w_routerT', [NE * D]); inp('w_moe_gate', [1, NE, D, FE]); inp('w_moe_up', [1, NE, D, FE])
        inp('w_moe_down', [1, NE, FE, D])
        inp('c32', list(a32.shape)); inp('cb', list(ab.shape), BF16)
        inp('cos', [128, L]); inp('sin', [128, L])
        self.yout = nc.dram_tensor("y", [L, D], F32, kind="ExternalOutput").ap()
        sc = self.scratch
        sc('xres', [T, D], F32)
        sc('gqT', [128, T], BF16); sc('gkT', [128, T], BF16); sc('gdT', [32, T], F32)
        sc('gktm', [T, 128], BF16); sc('gv', [T, 256], BF16); sc('gg', [T, 256], F32)
        sc('dqT', [8, 64, T], BF16); sc('dkT', [8, 64, T], BF16); sc('dva', [T, 4, 129], BF16)
        sc('nqT', [4, 64, T], BF16); sc('nkT', [4, 64, T], BF16); sc('nva', [T, 4, 65], BF16)
        sc('mix', [T, D], BF16); sc('h2T', [128, 8, T], BF16)
        sc('wfg', [D, FD], BF16, dbg=False); sc('wfu', [D, FD], BF16, dbg=False); sc('wfd', [FD, D], BF16, dbg=False)
        sc('wmg', [NE, D, FE], BF16, dbg=False); sc('wmu', [NE, D, FE], BF16, dbg=False); sc('wmd', [NE, FE, D], BF16, dbg=False)
        A = self.A
        self.c32 = A.alloc([a32.shape[1]], F32)
        self.cb = A.alloc([ab.shape[1]], BF16)
        self.dma('sp', self.c32, self.din['c32'][:, :], w=['c32'])
        self.dma('sp', self.cb, self.din['cb'][:, :], w=['cb'])
        self.k32 = {k: self.c32[:, o:o + n] for k, (o, n) in off32.items()}
        self.kb = {k: self.cb[:, o:o + n] for k, (o, n) in offb.items()}
        self.modL = A.alloc([6 * D], F32); self.modC = A.alloc([6 * D], F32)
        self.gates = A.alloc([32, 8], F32)
        self.S.barrier()
        self.pending_casts = []
        self.castkeys = {}
        if 0 in self.layers and self.stop_after is None:
            for nm, src in (('wfg', 'w_ffn_gate'), ('wfu', 'w_ffn_up'), ('wfd', 'w_ffn_down')):
                s_ = self.din[src][0].rearrange("(p a) f -> p a f", p=128)
                d_ = self.dsc[nm].rearrange("(p a) f -> p a f", p=128)
                na_ = s_.shape[1]
                for a0 in range(0, na_, max(1, na_ // 8)):
                    a1 = min(na_, a0 + max(1, na_ // 8))
                    self.pending_casts.append((d_[:, a0:a1, :], s_[:, a0:a1, :], f'@{nm}_{a0}'))
                    self.castkeys.setdefault('@' + nm, []).append(f'@{nm}_{a0}')
        if 1 in self.layers and self.stop_after is None:
            for e in range(NE):
                for nm, src in (('wmg', 'w_moe_gate'), ('wmu', 'w_moe_up'), ('wmd', 'w_moe_down')):
                    s_ = self.din[src][0, e].rearrange("(p a) f -> p a f", p=128)
                    d_ = self.dsc[nm][e].rearrange("(p a) f -> p a f", p=128)
                    na_ = s_.shape[1]
                    for a0 in range(0, na_, max(1, na_ // 8)):
                        a1 = min(na_, a0 + max(1, na_ // 8))
                        self.pending_casts.append((d_[:, a0:a1, :], s_[:, a0:a1, :], f'@{nm}{e}_{a0}'))
                        self.castkeys.setdefault(f'@{nm}{e}', []).append(f'@{nm}{e}_{a0}')

    def phase0_mod(self, l):
        A = self.A; m0 = A.mark()
        cc = A.alloc([8, 2], F32); sc_ = A.alloc([8, 2], F32)
        rep = A.alloc([16, 128], F32)
        self.dma('sp', cc, self.din['cc'][:, :, :], w=['cc'])
        self.act(sc_, cc, AF.Silu, ['cc'], ['sc'])
        for j in range(8):
            for v in range(2):
                self.ts('dve', rep[:, j * 2 + v, :], self.k32['ones'], sc_[:, j, v:v + 1], None, ALU.mult, None,
                        ['sc', 'c32'], [f'rep{j}{v}'])
        wm = [A.alloc([8, 512], F32) for _ in range(2)]
        bm_ = [A.alloc([512], F32) for _ in range(2)]
        for nb in range(12):
            b = nb % 2
            self.dma('sp', wm[b], self.din['w_mod'][l, :, nb * 512:(nb + 1) * 512].rearrange("(j p) n -> p j n", p=128),
                     w=[f'wm{b}'])
            self.dma('sp', bm_[b], self.din['b_mod'][l, nb * 512:(nb + 1) * 512].partition_broadcast(128), w=[f'bmod{b}'])
            for v, mod in enumerate((self.modL, self.modC)):
                pst = self.ps[v]
                for j in range(8):
                    self.mm(pst[:, :], rep[:, j * 2 + v, :], wm[b][:, j, :], j == 0, j == 7,
                            [f'rep{j}{v}', f'wm{b}'], [f'ps{v}'])
                self.tt('dve', mod[:, nb * 512:(nb + 1) * 512], pst[:, :], bm_[b], ALU.add,
                        [f'ps{v}', f'bmod{b}'], [f'mod{v}'])
        gB = A.alloc([D], F32)
        for gi, gname in enumerate(('g_norm1', 'g_norm2')):
            self.dma('sp', gB, self.din[gname][l].partition_broadcast(128), w=['gB'])
            o = 1024 + gi * 3072
            for v, mod in enumerate((self.modL, self.modC)):
                self.stt('dve', mod[:, o:o + 1024], mod[:, o:o + 1024], 1.0, gB, ALU.add, ALU.mult,
                         [f'mod{v}', 'gB'], [f'mod{v}'])
        self.S.barrier()
        A.reset(m0)

    def norm_mod(self, xt, mod, off, hout, kx, kss, kh, khout, ss, junk, htmp, kjunk='junk'):
        self.act(junk, xt, AF.Square, [kx], [kjunk, kss], accum_out=ss)
        self.rstd(ss, D, kss)
        self.stt('dve', htmp, xt, ss[:, 0:1], mod[:, off + 1024:off + 2048], ALU.mult, ALU.mult, [kx, kss, 'mod'], [kh])
        self.tt('pool', hout, htmp, mod[:, off:off + 1024], ALU.add, [kh, 'mod'], [khout])

    def transpose8(self, src_bf, dst, ksrc, kdst, bank=7):
        pv = self.ps[bank][:, :].bitcast(BF16)
        for j in range(8):
            self.tr(pv[:, j * 128:(j + 1) * 128], src_bf[:, j * 128:(j + 1) * 128], self.kb['ident'],
                    [ksrc, 'cb'], [f'ps{bank}'])
        self.cp('act', dst, pv.rearrange("p (j t) -> p j t", j=8), [f'ps{bank}'], [kdst])

    def phase1_inproj(self, l):
        A = self.A; m0 = A.mark()
        xsrc = self.din['xin'] if l == 0 else self.dsc['xres']
        win = A.alloc([8, INC], BF16)
        self.dma('pool', win, self.din['w_in'][l].rearrange("(j p) n -> p j n", p=128), w=['win'])
        cos = A.alloc([L], F32); sin = A.alloc([L], F32)
        self.dma('sp', cos, self.din['cos'][:, :], w=['cos'])
        self.dma('sp', sin, self.din['sin'][:, :], w=['sin'])
        xt = [A.alloc([D], F32) for _ in range(2)]
        htmp2 = [A.alloc([D], F32) for _ in range(2)]; junk_ = A.alloc([D], F32); junk2 = [junk_, junk_]
        hb = [A.alloc([D], BF16) for _ in range(2)]
        ss = [A.alloc([1], F32) for _ in range(2)]
        hT = [A.alloc([8, 512], BF16) for _ in range(2)]
        stg = [A.alloc([512], BF16) for _ in range(3)]
        stgd = A.alloc([512], F32)
        xb = [A.alloc([512], BF16) for _ in range(2)]
        t1 = [A.alloc([512], F32) for _ in range(2)]
        t2 = [A.alloc([512], F32) for _ in range(2)]
        skv = [A.alloc([384], BF16) for _ in range(2)]
        sgg = [A.alloc([256], F32) for _ in range(2)]
        sdv = [A.alloc([4, 129], BF16) for _ in range(2)]
        snv = [A.alloc([4, 65], BF16) for _ in range(2)]
        for b in range(2):
            self.memset('pool', sdv[b][:, :, 128:129], 1.0, [f'sdv{b}'])
            self.memset('pool', snv[b][:, :, 64:65], 1.0, [f'snv{b}'])
        chunks = [('gqT', None, 0, 128), ('gkT', None, 128, 128), ('gdT', None, 768, 32)]
        chunks += [('dqT', p_, 800 + p_ * 128, 128) for p_ in range(4)]
        chunks += [('dkT', p_, 1312 + p_ * 128, 128) for p_ in range(4)]
        chunks += [('nqT', p_, 2336 + p_ * 128, 128) for p_ in range(2)]
        chunks += [('nkT', p_, 2592 + p_ * 128, 128) for p_ in range(2)]
        nst = 0; nrope = 0; ntm = 0
        for tb in range(9):
            nsub = 4 if tb < 8 else 2
            N = nsub * 128; tok0 = tb * 512; isctx = tb == 8
            mod = self.modC if isctx else self.modL
            hTb = hT[tb % 2]; khT = f'hT{tb % 2}'
            for s in range(nsub):
                t = tb * 4 + s; b = t % 2
                self.dma('sp', xt[b], xsrc[t * 128:(t + 1) * 128, :], w=[f'xt{b}'])
                self.norm_mod(xt[b], mod, 0, hb[b], f'xt{b}', f'ss{b}', f'htmp{b}', f'hb{b}', ss[b], junk2[b], htmp2[b], kjunk='junk')
                self.transpose8(hb[b], hTb[:, :, s * 128:(s + 1) * 128], f'hb{b}', khT)
            for ci, (name, idx, c0, M) in enumerate(chunks):
                bank = ci % 3; pst = self.ps[bank]
                for j in range(8):
                    self.mm(pst[0:M, 0:N], win[:, j, c0:c0 + M], hTb[:, j, 0:N], j == 0, j == 7, ['win', khT], [f'ps{bank}'])
                dst = self.dsc[name]
                if name == 'gdT':
                    self.cp('dve', stgd[0:32, 0:N], pst[0:32, 0:N], [f'ps{bank}'], ['stgd'])
                    self.dma('pool', dst[0:32, tok0:tok0 + N], stgd[0:32, 0:N], r=['stgd'])
                    continue
                dap = dst[:, tok0:tok0 + N] if idx is None else \
                    dst.rearrange("a d t -> (a d) t")[idx * 128:(idx + 1) * 128, tok0:tok0 + N]
                sg = stg[nst % 3]; ksg = f'stg{nst % 3}'; nst += 1
                if name in ('dqT', 'dkT') and not isctx:
                    rb = nrope % 2; nrope += 1
                    self.cp('act', xb[rb][:, 0:N], pst[:, 0:N], [f'ps{bank}'], [f'xb{rb}'])
                    psr = self.ps[3 + rb]
                    self.mm(psr[:, 0:N], self.kb['RT'], xb[rb][:, 0:N], True, True, ['cb', f'xb{rb}'], [f'ps{3 + rb}'])
                    self.tt('dve', t1[rb][:, 0:N], xb[rb][:, 0:N], cos[:, tok0:tok0 + N], ALU.mult, [f'xb{rb}', 'cos'], [f't1{rb}'])
                    self.tt('dve', t2[rb][:, 0:N], psr[:, 0:N], sin[:, tok0:tok0 + N], ALU.mult, [f'ps{3 + rb}', 'sin'], [f't2{rb}'])
                    self.tt('pool', sg[:, 0:N], t1[rb][:, 0:N], t2[rb][:, 0:N], ALU.add, [f't1{rb}', f't2{rb}'], [ksg])
                else:
                    self.cp('act', sg[0:M, 0:N], pst[0:M, 0:N], [f'ps{bank}'], [ksg])
                self.dma('pool', dap, sg[0:M, 0:N], r=[ksg])
            for s in range(nsub):
                t = tb * 4 + s; b = ntm % 2; ntm += 1
                rows = slice(t * 128, (t + 1) * 128)
                lh = lambda j: hTb[:, j, s * 128:(s + 1) * 128]
                pa, pb_, pd, pn = self.ps[5], self.ps[6], self.ps[3 + (s % 2)], self.ps[6]
                for j in range(8):
                    self.mm(pa[:, 0:512], lh(j), win[:, j, 128:640], j == 0, j == 7, ['win', khT], ['ps5'])
                self.cp('dve', skv[b], pa[:, 0:384], ['ps5'], [f'skv{b}'])
                self.cp('dve', sgg[b][:, 0:128], pa[:, 384:512], ['ps5'], [f'sgg{b}'])
                for j in range(8):
                    self.mm(pb_[:, 0:128], lh(j), win[:, j, 640:768], j == 0, j == 7, ['win', khT], ['ps6'])
                self.cp('dve', sgg[b][:, 128:256], pb_[:, 0:128], ['ps6'], [f'sgg{b}'])
                self.dma('pool', self.dsc['gktm'][rows, :], skv[b][:, 0:128], r=[f'skv{b}'])
                self.dma('pool', self.dsc['gv'][rows, :], skv[b][:, 128:384], r=[f'skv{b}'])
                self.dma('pool', self.dsc['gg'][rows, :], sgg[b], r=[f'sgg{b}'])
                kpd = f'ps{3 + (s % 2)}'
                for j in range(8):
                    self.mm(pd[:, 0:512], lh(j), win[:, j, 1824:2336], j == 0, j == 7, ['win', khT], [kpd])
                self.cp('act', sdv[b][:, :, 0:128], pd[:, 0:512].rearrange("p (h j) -> p h j", h=4), [kpd], [f'sdv{b}'])
                self.dma('pool', self.dsc['dva'][rows, :, :], sdv[b], r=[f'sdv{b}'])
                for j in range(8):
                    self.mm(pn[:, 0:256], lh(j), win[:, j, 2848:3104], j == 0, j == 7, ['win', khT], ['ps6'])
                self.cp('dve', snv[b][:, :, 0:64], pn[:, 0:256].rearrange("p (h j) -> p h j", h=4), ['ps6'], [f'snv{b}'])
                self.dma('pool', self.dsc['nva'][rows, :, :], snv[b], r=[f'snv{b}'])
        self.S.barrier()
        A.reset(m0)

    def gla_dir(self, B, t, d, need_att, need_u):
        tok = slice(t * 128, (t + 1) * 128)
        kd = f'd{d}'
        U = self.k32['Uf' if d == 0 else 'Ub']; UX = self.k32['UfX' if d == 0 else 'UbX']
        ps = self.ps
        self.mm(ps[0][:, 0:128], B['gda'][0:33, tok], B['wupt'][0:33, d, :], True, True, ['gda', 'wupt'], ['ps0'])
        self.act(B['e1'], ps[0][:, 0:128], AF.Exp, ['ps0'], ['e1'], scale=-1.0)
        self.act(B['sp'][d], B['e1'], AF.Ln, ['e1'], ['sp' + kd], bias=1.0)
        self.mm(ps[1][:, 0:128], B['sp'][d], U, True, True, ['sp' + kd, 'c32'], ['ps1'])
        self.act(B['eb'][d], ps[1][:, 0:128], AF.Exp, ['ps1'], ['eb' + kd])
        if need_att:
            self.act(B['enb'], ps[1][:, 0:128], AF.Exp, ['ps1'], ['enb'], scale=-1.0)
            self.tt('dve', B['kinvT'], B['gkT'][:, tok], B['enb'], ALU.mult, ['gkT', 'enb'], ['kinvT'])
            for h in range(4):
                self.stt('dve', B['Qblk'][d][:, h, :], B['eb'][d], self.k32['maskh'][:, h:h + 1], B['gqT'][:, tok],
                         ALU.mult, ALU.mult, ['eb' + kd, 'c32', 'gqT'], ['Qblk' + kd])
            self.stt('dve', B['qdec'][d], B['eb'][d], 32.0 ** -0.5, B['gqT'][:, tok], ALU.mult, ALU.mult,
                     ['eb' + kd, 'gqT'], ['qdec' + kd])
            self.mm(ps[2][:, 0:512], B['kinvT'], B['Qblk'][d].rearrange("p h c -> p (h c)"), True, True,
                    ['kinvT', 'Qblk' + kd], ['ps2'])
            self.tt('dve', B['attM'][d].rearrange("p h c -> p (h c)"), ps[2][:, 0:512],
                    self.kb['maskF' if d == 0 else 'maskB'], ALU.mult, ['ps2', 'cb'], ['attM' + kd])
        if need_u:
            self.mm(ps[3][:, 0:128], UX, B['sp'][d], True, True, ['c32', 'sp' + kd], ['ps3'])
            self.act(B['ebd'], ps[3][:, 0:128], AF.Exp, ['ps3'], ['ebd'])
            self.tt('dve', B['kend'], B['gktm'][:, t, :], B['ebd'], ALU.mult, ['gktm', 'ebd'], ['kend'])
            self.mm(ps[4][:, 0:256], B['kend'], B['gv'][:, t, :], True, True, ['kend', 'gv'], ['ps4'])
            self.tt('dve', B['um'], ps[4][:, 0:256], self.k32['bm'], ALU.mult, ['ps4', 'c32'], ['um'])
            dec = B['eb'][d][:, 127:128] if d == 0 else B['eb'][d][:, 0:1]
            self.stt('dve', B['S'][d], B['S'][d], dec, B['um'], ALU.mult, ALU.add, ['S' + kd, 'eb' + kd, 'um'], ['S' + kd])

    def phase2_gla(self, l, last):
        A = self.A; m0 = A.mark(); B = {}
        B['gqT'] = A.alloc([T], BF16); B['gkT'] = A.alloc([T], BF16)
        B['gktm'] = A.alloc([NT, 128], BF16); B['gv'] = A.alloc([NT, 256], BF16)
        B['gda'] = A.alloc([T], F32); B['wupt'] = A.alloc([2, 128], F32)
        gnB = A.alloc([256], F32)
        SprevF = A.alloc([NT, 256], BF16)
        B['S'] = [A.alloc([256], F32) for _ in range(2)]
        Sbbf = [A.alloc([256], BF16) for _ in range(2)]
        B['e1'] = A.alloc([128], F32); B['sp'] = [A.alloc([128], F32) for _ in range(2)]
        B['eb'] = [A.alloc([128], F32) for _ in range(2)]; B['enb'] = A.alloc([128], F32)
        B['kinvT'] = A.alloc([128], BF16); B['Qblk'] = [A.alloc([4, 128], BF16) for _ in range(2)]
        B['qdec'] = [A.alloc([128], BF16) for _ in range(2)]; B['attM'] = [A.alloc([4, 128], BF16) for _ in range(2)]
        B['ebd'] = A.alloc([128], F32); B['kend'] = A.alloc([128], BF16); B['um'] = A.alloc([256], F32)
        osb = A.alloc([4, 64], F32); sq = A.alloc([4, 64], F32); ssq = A.alloc([4], F32); on = A.alloc([4, 64], F32)
        gg = [A.alloc([256], F32) for _ in range(2)]; sg = A.alloc([256], F32)
        stage = [A.alloc([256], BF16) for _ in range(2)]
        d = self.dsc
        self.dma('sp', B['gqT'], d['gqT'][:, :], w=['gqT']); self.dma('sp', B['gkT'], d['gkT'][:, :], w=['gkT'])
        for t0_ in range(0, NT, 8):
            t1_ = min(NT, t0_ + 8)
            self.dma('sp', B['gktm'][:, t0_:t1_, :], d['gktm'][t0_ * 128:t1_ * 128, :].rearrange("(t p) k -> p t k", p=128), w=['gktm'])
            self.dma('sp', B['gv'][:, t0_:t1_, :], d['gv'][t0_ * 128:t1_ * 128, :].rearrange("(t p) k -> p t k", p=128), w=['gv'])
        self.dma('sp', B['gda'][0:32], d['gdT'][:, :], w=['gda'])
        self.memset('pool', B['gda'][32:33], 1.0, ['gda'])
        self.dma('sp', B['wupt'][0:33], self.din['wup'][l].rearrange("d k n -> k d n"), w=['wupt'])
        self.dma('sp', gnB, self.din['gla_g'][l].partition_broadcast(128), w=['gnB'])
        for dd in range(2):
            self.memset('dve', B['S'][dd], 0.0, [f'Sd{dd}'])
        for t in [32, 33] + list(range(32)):
            self.cp('pool', SprevF[:, t, :], B['S'][0], ['Sd0'], [f'SprevF{t}'])
            self.gla_dir(B, t, 0, False, True)
        ps = self.ps
        for it, t in enumerate([33, 32] + list(range(31, -1, -1))):
            need_out = not (last and t >= 32)
            sb_ = Sbbf[it % 2]; ksb = f'Sbbf{it % 2}'
            self.cp('pool', sb_, B['S'][1], ['Sd1'], [ksb])
            if need_out:
                self.gla_dir(B, t, 0, True, False)
            self.gla_dir(B, t, 1, need_out, True)
            if not need_out:
                continue
            self.mm(ps[5][:, 0:256], B['qdec'][0], SprevF[:, t, :], True, False, ['qdecd0', f'SprevF{t}'], ['ps5'])
            self.mm(ps[5][:, 0:256], B['qdec'][1], sb_, False, False, ['qdecd1', ksb], ['ps5'])
            for dd in range(2):
                for h in range(4):
                    self.mm(ps[5][:, h * 64:(h + 1) * 64], B['attM'][dd][:, h, :], B['gv'][:, t, h * 64:(h + 1) * 64],
                            False, dd == 1 and h == 3, [f'attMd{dd}', 'gv'], ['ps5'])
            b = it % 2
            self.dma('sp', gg[b], d['gg'][t * 128:(t + 1) * 128, :], w=[f'gg{b}'])
            self.cp('act', osb.rearrange("p h c -> p (h c)"), ps[5][:, 0:256], ['ps5'], ['osb'])
            self.tt('dve', sq, osb, osb, ALU.mult, ['osb'], ['sq'])
            self.red('dve', ssq, sq, ALU.add, ['sq'], ['ssq'])
            self.rstd(ssq, 64, 'ssq')
            for h in range(4):
                self.stt('dve', on[:, h, :], osb[:, h, :], ssq[:, h:h + 1], gnB[:, h * 64:(h + 1) * 64], ALU.mult, ALU.mult,
                         ['osb', 'ssq', 'gnB'], ['on'])
            self.act(sg, gg[b], AF.Silu, [f'gg{b}'], ['sg'])
            self.tt('pool', stage[b], on.rearrange("p h c -> p (h c)"), sg, ALU.mult, ['on', 'sg'], [f'stage{b}'])
            self.dma('pool', d['mix'][t * 128:(t + 1) * 128, 0:256], stage[b], r=[f'stage{b}'])
        self.S.barrier()
        A.reset(m0)

    def phase3_diff(self, l, last):
        A = self.A; m0 = A.mark(); d = self.dsc; ps = self.ps
        lam_init = 0.8 - 0.6 * math.exp(-0.3 * l)
        lam4 = A.alloc([4, 64], F32); gdB = A.alloc([128], F32)
        self.dma('sp', lam4, self.din['diff_lambda'][l].partition_broadcast(128), w=['lam4'])
        self.dma('sp', gdB, self.din['diff_g'][l].partition_broadcast(128), w=['gdB'])
        lp = A.alloc([2, 64], F32); ls = A.alloc([2], F32); neglam = A.alloc([1], F32)
        self.tt('dve', lp, lam4[:, 0:4:2, :], lam4[:, 1:4:2, :], ALU.mult, ['lam4'], ['lp'])
        self.red('dve', ls, lp, ALU.add, ['lp'], ['ls'])
        self.act(ls, ls, AF.Exp, ['ls'], ['ls'])
        self.tt('dve', neglam, ls[:, 1:2], ls[:, 0:1], ALU.subtract, ['ls'], ['neglam'])
        self.ts('dve', neglam, neglam, -lam_init, None, ALU.add, None, ['neglam'], ['neglam'])
        QT = [A.alloc([T], BF16) for _ in range(2)]; KT = [A.alloc([T], BF16) for _ in range(2)]
        VA = [A.alloc([NT, 129], BF16) for _ in range(2)]
        PT = [A.alloc([512], BF16) for _ in range(4)]
        den = A.alloc([2], F32); t0 = A.alloc([128], F32); a_ = A.alloc([128], F32); junk = A.alloc([128], F32)
        ssd = A.alloc([1], F32); stage = [A.alloc([4, 128], BF16) for _ in range(2)]
        zl = A.alloc([128], BF16); zr = A.alloc([512], BF16)
        self.memset('pool', zl, 0.0, ['zl']); self.memset('pool', zr, 0.0, ['zr'])
        nq = 0; npt = 0
        for h in range(4):
            b = h % 2
            self.dma('sp', QT[b], d['dqT'][2 * h:2 * h + 2].rearrange("m d t -> (m d) t"), w=[f'QT{b}'])
            self.dma('sp', KT[b], d['dkT'][2 * h:2 * h + 2].rearrange("m d t -> (m d) t"), w=[f'KT{b}'])
            for t0_ in range(0, NT, 8):
                t1_ = min(NT, t0_ + 8)
                self.dma('sp', VA[b][:, t0_:t1_, :], d['dva'][t0_ * 128:t1_ * 128, h, :].rearrange("(t p) j -> p t j", p=128), w=[f'VA{b}'])
            qblocks = [(qb * 512, 512, list(range(34))) for qb in range(8)]
            if not last:
                qblocks.append((4096, 256, [32, 33]))
            for (q0, N, kcs) in qblocks:
                nsub = N // 128
                for bo in sorted(set(4 + 2 * m + s_ // 2 for m in range(2) for s_ in range(nsub))):
                    self.mm(ps[bo][:, 0:512], zl, zr, True, False, ['zl', 'zr'], [f'ps{bo}'])
                steps = [(ki, kc, m) for ki, kc in enumerate(kcs) for m in range(2)]

                def s_step(j, npt_):
                    ki, kc, m = steps[j]
                    bank = j % 4
                    self.mm(ps[bank][:, 0:N], KT[b][64 * m:64 * m + 64, kc * 128:(kc + 1) * 128], QT[b][64 * m:64 * m + 64, q0:q0 + N],
                            True, True, [f'KT{b}', f'QT{b}'], [f'ps{bank}'])
                    pt = PT[npt_ % 4]; kpt = f'PT{npt_ % 4}'
                    self.act(pt[:, 0:N], ps[bank][:, 0:N], AF.Exp, [f'ps{bank}'], [kpt], scale=0.125)
                    return pt, kpt

                def pv_step(j, pt, kpt):
                    ki, kc, m = steps[j]
                    for s in range(nsub):
                        bo = 4 + 2 * m + s // 2; c0 = (s % 2) * 256
                        self.mm(ps[bo][:, c0:c0 + 129], pt[:, s * 128:(s + 1) * 128], VA[b][:, kc, :],
                                False, ki == len(kcs) - 1 and s % 2 == 1, [kpt, f'VA{b}'], [f'ps{bo}'])

                prev = None
                for j in range(0, len(steps), 2):
                    cur0 = s_step(j, npt); npt += 1
                    cur1 = s_step(j + 1, npt); npt += 1
                    if prev is not None:
                        pv_step(*prev[0]); pv_step(*prev[1])
                    prev = ((j,) + cur0, (j + 1,) + cur1)
                pv_step(*prev[0]); pv_step(*prev[1])
                st = stage[nq % 2]; kst = f'dstage{nq % 2}'; nq += 1
                for s in range(nsub):
                    c0 = (s % 2) * 256
                    o0 = ps[4 + s // 2][:, c0:c0 + 129]; o1 = ps[6 + s // 2][:, c0:c0 + 129]
                    k0 = f'ps{4 + s // 2}'; k1 = f'ps{6 + s // 2}'
                    self.cp('dve', den[:, 0:1], o0[:, 128:129], [k0], ['den'])
                    self.cp('dve', den[:, 1:2], o1[:, 128:129], [k1], ['den'])
                    self.recip(den, den, ['den'], ['den'])
                    self.tt('dve', den[:, 1:2], den[:, 1:2], neglam, ALU.mult, ['den', 'neglam'], ['den'])
                    self.ts('dve', t0, o0[:, 0:128], den[:, 0:1], None, ALU.mult, None, [k0, 'den'], ['t0'])
                    self.stt('dve', a_, o1[:, 0:128], den[:, 1:2], t0, ALU.mult, ALU.add, [k1, 'den', 't0'], ['a_'])
                    self.act(junk, a_, AF.Square, ['a_'], ['djunk', 'ssd'], accum_out=ssd)
                    self.rstd(ssd, 128, 'ssd')
                    self.ts('dve', ssd, ssd, 1.0 - lam_init, None, ALU.mult, None, ['ssd'], ['ssd'])
                    self.stt('dve', st[:, s, :], a_, ssd[:, 0:1], gdB, ALU.mult, ALU.mult, ['a_', 'ssd', 'gdB'], [kst])
                self.dma('pool', d['mix'][q0:q0 + N, 256 + h * 128:256 + (h + 1) * 128].rearrange("(s p) j -> p s j", p=128),
                         st[:, 0:nsub, :], r=[kst])
        self.S.barrier()
        A.reset(m0)

    def phase4_na(self, l, last):
        A = self.A; m0 = A.mark(); d = self.dsc; ps = self.ps
        QT = [A.alloc([T], BF16) for _ in range(2)]; KT = [A.alloc([T], BF16) for _ in range(2)]
        VA = [A.alloc([NT, 65], BF16) for _ in range(2)]; VB = [A.alloc([31, 65], BF16) for _ in range(2)]
        bias = [A.alloc([14, 64], F32) for _ in range(2)]
        tmp = [A.alloc([4, 64], F32) for _ in range(2)]; P = [A.alloc([6, 64], BF16) for _ in range(3)]
        den = A.alloc([4], F32); stage = [A.alloc([4, 64], BF16) for _ in range(2)]
        P2 = [A.alloc([256], BF16) for _ in range(2)]; st2 = [A.alloc([64], BF16) for _ in range(2)]
        nonlocal_ng = [0]
        for h in range(4):
            b = h % 2
            self.dma('sp', QT[b][0:64], d['nqT'][h], w=[f'nQT{b}'])
            self.dma('sp', KT[b][0:64], d['nkT'][h], w=[f'nKT{b}'])
            for t0_ in range(0, NT, 8):
                t1_ = min(NT, t0_ + 8)
                self.dma('sp', VA[b][:, t0_:t1_, :], d['nva'][t0_ * 128:t1_ * 128, h, :].rearrange("(t p) j -> p t j", p=128), w=[f'nVA{b}'])
            for t0_ in range(0, 31, 8):
                t1_ = min(31, t0_ + 8)
                self.dma('sp', VB[b][:, t0_:t1_, :], d['nva'][64 + t0_ * 128:64 + t1_ * 128, h, :].rearrange("(t p) j -> p t j", p=128), w=[f'nVB{b}'])
            self.dma('sp', bias[b], self.din['nabias'][l, h], w=[f'nbias{b}'])
            rd = [f'nKT{b}', f'nQT{b}']
            def na_s(r):
                rs = min(max(r - 4, 0), 56); d0i = rs - r + 7
                bs = r % 3; kbs = f'ps{bs}'
                psS = ps[bs][:, 0:384].rearrange("p (i q) -> p i q", i=6)
                qs = QT[b][0:64, r * 64:(r + 1) * 64]
                for i in range(6):
                    o_i = (rs + 2 * i) * 64 if i < 4 else 4096 + (i - 4) * 128
                    self.mm(psS[:, i, :], KT[b][0:64, o_i:o_i + 128], qs, True, True, rd, [kbs])
                tb_ = tmp[r % 2]; pb = P[r % 3]
                self.stt('dve', tb_, psS[:, 0:4, :], 0.125, bias[b][:, d0i:d0i + 7:2, :], ALU.mult, ALU.add,
                         [kbs, f'nbias{b}'], [f'ntmp{r % 2}'])
                self.act(pb[:, 0:4, :], tb_, AF.Exp, [f'ntmp{r % 2}'], [f'nP{r % 3}'])
                self.act(pb[:, 4:6, :], psS[:, 4:6, :], AF.Exp, [kbs], [f'nP{r % 3}'], scale=0.125)

            def na_pv(r):
                rg = r // 4; r4 = r % 4
                bo = 4 + rg % 2; kbo = f'ps{bo}'
                rs = min(max(r - 4, 0), 56)
                pb = P[r % 3]
                for i in range(6):
                    if i < 4:
                        ro = rs + 2 * i
                        V = VA[b][:, ro // 2, :] if ro % 2 == 0 else VB[b][:, (ro - 1) // 2, :]
                    else:
                        V = VA[b][:, 32 + (i - 4), :]
                    self.mm(ps[bo][0:64, r4 * 65:(r4 + 1) * 65], pb[:, i, :], V, i == 0, i == 5,
                            [f'nP{r % 3}', f'nVA{b}', f'nVB{b}'], [kbo])
                if r4 == 3:
                    nonlocal_ng[0] += 1
                    pso = ps[bo][0:64, 0:260].rearrange("p (r j) -> p r j", r=4)
                    st = stage[nonlocal_ng[0] % 2]; kst = f'nstage{nonlocal_ng[0] % 2}'
                    self.cp('dve', den[0:64], pso[:, :, 64], [kbo], ['nden'])
                    self.recip(den[0:64], den[0:64], ['nden'], ['nden'])
                    for q4 in range(4):
                        self.ts('dve', st[0:64, q4, :], pso[:, q4, 0:64], den[0:64, q4:q4 + 1], None, ALU.mult, None,
                                [kbo, 'nden'], [kst])
                    self.dma('pool', d['mix'][rg * 256:(rg + 1) * 256, 768 + h * 64:768 + (h + 1) * 64].rearrange("(r p) j -> p r j", p=64),
                             st[0:64], r=[kst])

            for r in range(64):
                na_s(r)
                if r > 0:
                    na_pv(r - 1)
            na_pv(63)
            if not last:
                qc = QT[b][0:64, 4096:4352]
                for i in range(2):
                    self.mm(ps[i][:, 0:256], KT[b][0:64, 4096 + i * 128:4096 + (i + 1) * 128], qc, True, True, rd, [f'ps{i}'])
                    self.act(P2[i], ps[i][:, 0:256], AF.Exp, [f'ps{i}'], [f'nP2{i}'], scale=0.125)
                for qs_ in range(2):
                    bo = 4 + qs_; kbo = f'ps{bo}'
                    for i in range(2):
                        self.mm(ps[bo][:, 0:65], P2[i][:, qs_ * 128:(qs_ + 1) * 128], VA[b][:, 32 + i, :], i == 0, i == 1,
                                [f'nP2{i}', f'nVA{b}'], [kbo])
                    self.cp('dve', den[:, 0:1], ps[bo][:, 64:65], [kbo], ['nden'])
                    self.recip(den[:, 0:1], den[:, 0:1], ['nden'], ['nden'])
                    self.ts('dve', st2[qs_], ps[bo][:, 0:64], den[:, 0:1], None, ALU.mult, None, [kbo, 'nden'], [f'nst2{qs_}'])
                    self.dma('pool', d['mix'][4096 + qs_ * 128:4096 + (qs_ + 1) * 128, 768 + h * 64:768 + (h + 1) * 64],
                             st2[qs_], r=[f'nst2{qs_}'])
        self.S.barrier()
        A.reset(m0)

    def phase5_outproj(self, l, last):
        A = self.A; m0 = A.mark(); d = self.dsc; ps = self.ps
        moe = (l % 2 == 1)
        xsrc = self.din['xin'] if l == 0 else d['xres']
        wout = A.alloc([8, D], BF16)
        self.dma('pool', wout, self.din['w_out'][l].rearrange("(j p) n -> p j n", p=128), w=['wout'])
        if moe:
            wrB = A.alloc([8, D], F32)
            self.dma('sp', wrB.rearrange("p e n -> p (e n)"), self.din['w_routerT'].partition_broadcast(128), w=['wrB'])
            junkr = A.alloc([D], F32); lg = A.alloc([8], F32); l2 = A.alloc([8], F32)
            eq1 = A.alloc([8], F32); eq2 = A.alloc([8], F32); sm = A.alloc([4], F32)
        mixt = [A.alloc([D], BF16) for _ in range(2)]; mixT = [A.alloc([8, 128], BF16) for _ in range(2)]
        xt = [A.alloc([D], F32) for _ in range(2)]; tmp2 = [A.alloc([D], F32) for _ in range(2)]; xn = [A.alloc([D], F32) for _ in range(2)]
        junk2 = [A.alloc([D], F32) for _ in range(2)]; ss = [A.alloc([1], F32) for _ in range(2)]; htmp2 = [A.alloc([D], F32) for _ in range(2)]; h2f2 = [A.alloc([D], F32) for _ in range(2)]
        h2b = [A.alloc([D], BF16) for _ in range(2)]; h2Ts = [A.alloc([8, 128], BF16) for _ in range(2)]
        ntiles = 32 if last else 34
        for t in range(ntiles):
            b = t % 2; rows = slice(t * 128, (t + 1) * 128); isctx = t >= 32
            mod = self.modC if isctx else self.modL
            tmp = tmp2[b]; junk = junk2[b]; htmp = htmp2[b]; h2f = h2f2[b]
            self.dma('sp', mixt[b], d['mix'][rows, :], w=[f'mixt{b}'])
            self.dma('sp', xt[b], xsrc[rows, :], w=[f'oxt{b}'])
            self.transpose8(mixt[b], mixT[b], f'mixt{b}', f'mixT{b}', bank=7)
            for half in range(2):
                for j in range(8):
                    self.mm(ps[half][:, 0:512], mixT[b][:, j, :], wout[:, j, half * 512:(half + 1) * 512], j == 0, j == 7,
                            [f'mixT{b}', 'wout'], [f'ps{half}'])
                self.tt('dve', tmp[:, half * 512:(half + 1) * 512], ps[half][:, 0:512],
                        mod[:, 2048 + half * 512:2048 + (half + 1) * 512], ALU.mult, [f'ps{half}'], [f'otmp{b}'])
            self.tt('pool', xn[b], xt[b], tmp, ALU.add, [f'oxt{b}', f'otmp{b}'], [f'xn{b}'])
            self.dma('pool', d['xres'][rows, :], xn[b], r=[f'xn{b}'])
            self.norm_mod(xn[b], mod, 3072, h2f, f'xn{b}', f'oss{b}', f'ohtmp{b}', f'h2f{b}', ss[b], junk, htmp, kjunk=f'ojunk{b}')
            self.cp('act', h2b[b], h2f, [f'h2f{b}'], [f'h2b{b}'])
            self.transpose8(h2b[b], h2Ts[b], f'h2b{b}', f'h2Ts{b}', bank=6)
            self.dma('pool', d['h2T'][:, :, rows], h2Ts[b], r=[f'h2Ts{b}'])
            if moe and not isctx:
                self.memset('dve', lg, 0.0, ['lg'])
                for e in range(NE):
                    self.stt('dve', junkr, h2f, 1.0, wrB[:, e, :], ALU.mult, ALU.mult, [f'h2f{b}', 'wrB'], ['junkr', 'lg'],
                             accum_out=lg[:, e:e + 1])
                m1 = sm[:, 0:1]; m2 = sm[:, 1:2]; ee = sm[:, 2:3]; w1 = sm[:, 3:4]
                self.red('dve', m1, lg, ALU.max, ['lg'], ['sm'])
                self.ts('dve', eq1, lg, m1, None, ALU.is_equal, None, ['lg', 'sm'], ['eq1'])
                self.stt('dve', l2, eq1, -1e30, lg, ALU.mult, ALU.add, ['eq1', 'lg'], ['l2'])
                self.red('dve', m2, l2, ALU.max, ['l2'], ['sm'])
                self.ts('dve', eq2, l2, m2, None, ALU.is_equal, None, ['l2', 'sm'], ['eq2'])
                self.tt('dve', ee, m2, m1, ALU.subtract, ['sm'], ['sm'])
                self.act(ee, ee, AF.Exp, ['sm'], ['sm'])
                self.ts('dve', w1, ee, 1.0, None, ALU.add, None, ['sm'], ['sm'])
                self.recip(w1, w1, ['sm'], ['sm'])
                self.tt('dve', ee, ee, w1, ALU.mult, ['sm'], ['sm'])
                self.ts('dve', self.gates[:, t, :], eq1, w1, None, ALU.mult, None, ['eq1', 'sm'], ['@gates'])
                self.stt('dve', self.gates[:, t, :], eq2, ee, self.gates[:, t, :], ALU.mult, ALU.add, ['eq2', 'sm', '@gates'], ['@gates'])
        self.S.barrier()
        A.reset(m0)

    def phase6_ffn(self, l, last):
        A = self.A; m0 = A.mark(); d = self.dsc; ps = self.ps
        moe = (l % 2 == 1)
        nblocks = 8 if last else 9
        if moe:
            nexp = NE; F = FE
            wg = lambda e: d['wmg'][e]; wu = lambda e: d['wmu'][e]; wd = lambda e: d['wmd'][e]
            kg = lambda e: f'@wmg{e}'; ku = lambda e: f'@wmu{e}'; kd = lambda e: f'@wmd{e}'
        else:
            nexp = 1; F = FD
            wg = lambda e: d['wfg']; wu = lambda e: d['wfu']; wd = lambda e: d['wfd']
            kg = lambda e: '@wfg'; ku = lambda e: '@wfu'; kd = lambda e: '@wfd'
        nfc = F // 128
        groups = [(c0, min(4, nfc - c0)) for c0 in range(0, nfc, 4)]
        h2T = [A.alloc([8, 512], BF16) for _ in range(2)]
        Wg = [A.alloc([8, 512], BF16) for _ in range(3)]; Wu = [A.alloc([8, 512], BF16) for _ in range(3)]
        Wd = [A.alloc([4, D], BF16) for _ in range(3)]
        sgl = [A.alloc([512], F32) for _ in range(2)]; actT = [A.alloc([4, 512], BF16) for _ in range(2)]
        yacc = A.alloc([4, D], F32)
        xt = [A.alloc([D], F32) for _ in range(2)]; tmp = A.alloc([D], F32); xn_ = A.alloc([D], F32); xn = [xn_, xn_]
        junk = A.alloc([D], F32); ss = A.alloc([1], F32); outs_ = A.alloc([D], F32); outs = [outs_, outs_]
        if last:
            gfB = A.alloc([D], F32)
            self.dma('sp', gfB, self.din['g_final'].partition_broadcast(128), w=['gfB'])
        nw = 0; na = 0; nt = 0
        for tb in range(nblocks):
            nsub = 4 if tb < 8 else 2
            N = nsub * 128; tok0 = tb * 512; isctx = tb == 8
            mod = self.modC if isctx else self.modL
            hb = h2T[tb % 2]; khb = f'fh2T{tb % 2}'
            self.dma('sp', hb[:, :, 0:N], d['h2T'][:, :, tok0:tok0 + N], w=[khb])
            def gu(e, c0, nf, nw_, na_):
                wb = nw_ % 3
                fs = slice(c0 * 128, (c0 + nf) * 128)
                self.dma('sp', Wg[wb][:, :, 0:nf * 128], wg(e)[:, fs].rearrange("(j p) f -> p j f", p=128), r=self.castkeys[kg(e)], w=[f'Wg{wb}'])
                self.dma('sp', Wu[wb][:, :, 0:nf * 128], wu(e)[:, fs].rearrange("(j p) f -> p j f", p=128), r=self.castkeys[ku(e)], w=[f'Wu{wb}'])
                self.dma('sp', Wd[wb][:, 0:nf, :], wd(e)[fs, :].rearrange("(c p) n -> p c n", p=128), r=self.castkeys[kd(e)], w=[f'Wd{wb}'])
                at = actT[na_ % 2]; kat = f'actT{na_ % 2}'
                for c in range(nf):
                    bg = c % 2; bu = 2 + c % 2
                    for j in range(8):
                        self.mm(ps[bg][:, 0:N], Wg[wb][:, j, c * 128:(c + 1) * 128], hb[:, j, 0:N], j == 0, j == 7,
                                [f'Wg{wb}', khb], [f'ps{bg}'])
                    for j in range(8):
                        self.mm(ps[bu][:, 0:N], Wu[wb][:, j, c * 128:(c + 1) * 128], hb[:, j, 0:N], j == 0, j == 7,
                                [f'Wu{wb}', khb], [f'ps{bu}'])
                    self.act(sgl[c % 2][:, 0:N], ps[bg][:, 0:N], AF.Silu, [f'ps{bg}'], [f'sgl{c % 2}'])
                    self.tt('dve', at[:, c, 0:N], sgl[c % 2][:, 0:N], ps[bu][:, 0:N], ALU.mult, [f'sgl{c % 2}', f'ps{bu}'], [kat])
                return (e, nf, wb, at, kat)

            def down(e, nf, wb, at, kat, first):
                for s in range(nsub):
                    for half in range(2):
                        by = 4 + (s * 2 + half) % 4
                        for c in range(nf):
                            self.mm(ps[by][:, 0:512], at[:, c, s * 128:(s + 1) * 128], Wd[wb][:, c, half * 512:(half + 1) * 512],
                                    c == 0, c == nf - 1, [kat, f'Wd{wb}'], [f'ps{by}'])
                        ya = yacc[:, s, half * 512:(half + 1) * 512]
                        if moe:
                            g_ = self.gates[:, tb * 4 + s, e:e + 1]
                            if first:
                                self.ts('dve', ya, ps[by][:, 0:512], g_, None, ALU.mult, None, [f'ps{by}', '@gates'], ['yacc'])
                            else:
                                self.stt('dve', ya, ps[by][:, 0:512], g_, ya, ALU.mult, ALU.add, [f'ps{by}', '@gates', 'yacc'], ['yacc'])
                        else:
                            if first:
                                self.cp('dve', ya, ps[by][:, 0:512], [f'ps{by}'], ['yacc'])
                            else:
                                self.tt('dve', ya, ps[by][:, 0:512], ya, ALU.add, [f'ps{by}', 'yacc'], ['yacc'])

            items = [(e, c0, nf) for e in range(nexp) for (c0, nf) in groups]
            prev = None; first = True
            for (e, c0, nf) in items:
                cur = gu(e, c0, nf, nw, na); nw += 1; na += 1
                if prev is not None:
                    down(*prev, first); first = False
                prev = cur
            down(*prev, first)
            for s in range(nsub):
                t = tb * 4 + s; b = nt % 2; nt += 1
                rows = slice(t * 128, (t + 1) * 128)
                self.dma('sp', xt[b], d['xres'][rows, :], w=[f'fxt{b}'])
                self.tt('dve', tmp, yacc[:, s, :], mod[:, 5120:6144], ALU.mult, ['yacc'], ['ftmp'])
                self.tt('pool', xn[b], xt[b], tmp, ALU.add, [f'fxt{b}', 'ftmp'], ['fxn'])
                if last:
                    self.act(junk, xn[b], AF.Square, ['fxn'], ['fjunk', 'fss'], accum_out=ss)
                    self.rstd(ss, D, 'fss')
                    self.stt('dve', outs[b], xn[b], ss[:, 0:1], gfB, ALU.mult, ALU.mult, ['fxn', 'fss', 'gfB'], ['outs'])
                    self.dma('pool', self.yout[rows, :], outs[b], r=['outs'])
                else:
                    self.dma('pool', d['xres'][rows, :], xn[b], r=['fxn'])
        self.S.barrier()
        A.reset(m0)

    def build(self):
        self.setup()
        for l in self.layers:
            last = (l == DEPTH - 1)
            self.phase0_mod(l)
            self.phase1_inproj(l)
            if self.stop_after == 'inproj':
                break
            self.phase2_gla(l, last)
            if self.stop_after == 'gla':
                break
            self.phase3_diff(l, last)
            if self.stop_after == 'diff':
                break
            self.phase4_na(l, last)
            if self.stop_after == 'na':
                break
            self.phase5_outproj(l, last)
            if self.stop_after == 'outproj':
                break
            self.flush_casts('@wf' if l == 0 else '@wm')
            self.phase6_ffn(l, last)
        self.S.emit()
        return self.nc


def _host_inputs(inputs):
    f = lambda k: np.asarray(inputs[k], dtype=np.float32)
    a32, off32, ab, offb, cos, sin = _consts()
    wup = np.zeros((DEPTH, 2, 33, 128), np.float32)
    wdu = f('gla_w_dec_up'); bde = f('gla_b_dec')
    wup[:, 0, 0:16] = wdu[:, 0]; wup[:, 1, 16:32] = wdu[:, 1]
    wup[:, 0, 32] = bde[:, 0]; wup[:, 1, 32] = bde[:, 1]
    shared = {
        'w_mod': f('w_mod'), 'b_mod': f('b_mod'), 'g_norm1': f('g_norm1'), 'g_norm2': f('g_norm2'),
        'g_final': f('g_final'), 'w_in': f('w_in'), 'wup': wup,
        'gla_g': np.ascontiguousarray(np.tile(f('gla_g_norm'), (1, 4))),
        'diff_lambda': np.ascontiguousarray(f('diff_lambda').reshape(DEPTH, 256)),
        'diff_g': f('diff_g_norm'), 'nabias': _na_bias(f('na_rpb')), 'w_out': f('w_out'),
        'w_ffn_gate': f('w_ffn_gate'), 'w_ffn_up': f('w_ffn_up'), 'w_ffn_down': f('w_ffn_down'),
        'w_routerT': np.ascontiguousarray(f('w_router')[0].T).reshape(-1),
        'w_moe_gate': f('w_moe_gate'), 'w_moe_up': f('w_moe_up'), 'w_moe_down': f('w_moe_down'),
        'c32': a32, 'cb': ab, 'cos': cos, 'sin': sin,
    }
    x = f('x'); ctx = f('ctx'); c = f('c'); cctx = f('c_ctx')
    maps = []
    for b in range(8):
        m = dict(shared)
        m['xin'] = np.ascontiguousarray(np.concatenate([x[b], ctx[b]], axis=0))
        cc = np.stack([c[b], cctx], axis=-1)
        m['cc'] = np.ascontiguousarray(cc.reshape(8, 128, 2).transpose(1, 0, 2))
        maps.append(m)
    return maps


_NC_CACHE = {}


def kernel(**inputs):
    if 'nc' not in _NC_CACHE:
        _NC_CACHE['nc'] = K().build()
    nc = _NC_CACHE['nc']
    maps = _host_inputs(inputs)
    res = run_bass_kernel_spmd(nc, maps, core_ids=list(range(8)))
    return np.stack([np.asarray(r['y'], dtype=np.float32) for r in res.results], axis=0)
```

```python
import contextlib, math
import numpy as np
import ml_dtypes
import concourse.bass as bass
import concourse.mybir as mybir
from concourse.bass_utils import run_bass_kernel_spmd

F32 = mybir.dt.float32
BF16 = mybir.dt.bfloat16
AF = mybir.ActivationFunctionType
ALU = mybir.AluOpType
AX = mybir.AxisListType

L = 4096; C = 256; T = 4352; D = 1024; NT = 34; DEPTH = 2
INC = 3104
FD = 2816; FE = 3584; NE = 8
EPS = 1e-6
NEG = -30000.0

SAME_ENG_SYNC = True
BANK = 30000
NDMA = 8


class Sched:
    ENGS = ['pe', 'dve', 'act', 'pool', 'sp']

    def __init__(self, nc, es):
        self.nc = nc
        self.es = es
        self.ops = []
        self.lastw = {}
        self.readers = {}
        self.dmahist = {e: [] for e in self.ENGS}
        self.last_on = {e: None for e in self.ENGS}
        self.nown = 0
        self.casthist = []

    def _deps(self, reads, writes):
        deps = set()
        for k in reads:
            w = self.lastw.get(k)
            if w is not None:
                deps.add(w)
        for k in writes:
            w = self.lastw.get(k)
            if w is not None:
                deps.add(w)
            for r in self.readers.get(k, ()):
                deps.add(r)
        return deps

    def _prune(self, deps):
        best = {}
        out = set()
        for d in deps:
            od = self.ops[d]
            if od['kind'] == 'op':
                e = od['eng']
                if e not in best or d > best[e]:
                    best[e] = d
            else:
                out.add(d)
        out.update(best.values())
        return out

    def _commit(self, idx, reads, writes):
        for k in reads:
            self.readers.setdefault(k, []).append(idx)
        for k in writes:
            self.lastw[k] = idx
            self.readers[k] = []

    def op(self, eng, fn, reads=(), writes=()):
        deps = self._prune(self._deps(reads, writes))
        idx = len(self.ops)
        self.ops.append(dict(eng=eng, fn=fn, deps=deps, kind='op', inc=False))
        self._commit(idx, reads, writes)
        self.last_on[eng] = idx
        return idx

    def dma(self, q, out, in_, reads=(), writes=(), own=False):
        deps = self._prune(self._deps(reads, writes))
        idx = len(self.ops)
        o = dict(eng=q, fn=(lambda e, o=out, i=in_: e.dma_start(out=o, in_=i)),
                 deps=deps, kind='dma', inc=True)
        if own:
            h = self.casthist
            if len(h) >= 2:
                deps.add(h[-2])
            o['cn'] = len(h)
            h.append(idx)
        else:
            h = self.dmahist[q]
            if len(h) >= NDMA:
                deps.add(h[-NDMA])
            o['n'] = len(h)
            h.append(idx)
        self.ops.append(o)
        self._commit(idx, reads, writes)
        return idx

    def _alltoks(self):
        toks = set()
        for e in self.ENGS:
            if self.last_on[e] is not None:
                toks.add(self.last_on[e])
            for d in self.dmahist[e][-NDMA:]:
                toks.add(d)
        return toks

    def barrier(self):
        toks = self._alltoks()
        for e in self.ENGS:
            self.ops.append(dict(eng=e, fn=None, deps=set(toks), kind='bar', inc=False))
        self.lastw = {k: v for k, v in self.lastw.items() if isinstance(k, str) and k.startswith('@')}
        self.readers = {k: v for k, v in self.readers.items() if isinstance(k, str) and k.startswith('@')}

    def emit(self, final_wait_eng='sp'):
        nc = self.nc
        ops = self.ops
        toks = self._alltoks()
        for i_ in self.casthist[-2:]:
            toks.add(i_)
        ops.append(dict(eng=final_wait_eng, fn=None, deps=toks, kind='bar', inc=False))
        for o in ops:
            for d in o['deps']:
                od = ops[d]
                if od['kind'] == 'op':
                    if od['eng'] == o['eng'] and (od['eng'] == 'pe' or not SAME_ENG_SYNC):
                        continue
                    od['inc'] = True
        cnt = {e: 0 for e in self.ENGS}
        for o in ops:
            if o['kind'] == 'op' and o['inc']:
                c = cnt[o['eng']]
                o['tok'] = (o['eng'], c // BANK, c % BANK + 1)
                cnt[o['eng']] = c + 1
            elif o['kind'] == 'dma':
                if 'cn' in o:
                    o['tok'] = ('cast', o['cn'] % 2, 16 * (o['cn'] // 2 + 1))
                else:
                    n = o['n']
                    o['tok'] = ('d' + o['eng'], n % NDMA, 16 * (n // NDMA + 1))
        sems = {}

        def getsem(name, bank):
            key = (name, bank)
            if key not in sems:
                sems[key] = self.es.enter_context(nc.semaphore(f"s_{name}_{bank}"))
            return sems[key]

        for e in self.ENGS:
            for b in range(cnt[e] // BANK + 1):
                getsem(e, b)
            if self.dmahist[e]:
                for s in range(NDMA):
                    getsem('d' + e, s)
        if self.casthist:
            getsem('cast', 0); getsem('cast', 1)
        blk = self.es.enter_context(nc.Block())
        nwaits = {}
        self.nwaits = nwaits

        def run(ename):
            def body(e):
                seen = {}
                for o in ops:
                    if o['eng'] != ename:
                        continue
                    for d in sorted(o['deps']):
                        od = ops[d]
                        if 'tok' not in od:
                            continue
                        if od['kind'] == 'op' and od['eng'] == ename and (ename == 'pe' or not SAME_ENG_SYNC):
                            continue
                        name, bank, val = od['tok']
                        if seen.get((name, bank), 0) >= val:
                            continue
                        e.wait_ge(getsem(name, bank), val)
                        nwaits[ename] = nwaits.get(ename, 0) + 1
                        seen[(name, bank)] = val
                    if o['fn'] is None:
                        continue
                    inst = o['fn'](e)
                    if o['kind'] == 'dma':
                        name, bank, val = o['tok']
                        inst.then_inc(getsem(name, bank), 16)
                    elif o['inc']:
                        name, bank, val = o['tok']
                        inst.then_inc(getsem(name, bank), 1)
            return body

        blk.tensor(run('pe'))
        blk.vector(run('dve'))
        blk.scalar(run('act'))
        blk.gpsimd(run('pool'))
        blk.sync(run('sp'))
        self.stats = dict(nops=len(ops), cnt=cnt, nsem=len(sems))


class Arena:
    def __init__(self, nc, es, nwords):
        self.t = es.enter_context(nc.sbuf_tensor("arena", [128, nwords], F32))
        self.n = nwords
        self.off = 0

    def mark(self):
        return self.off

    def reset(self, m):
        self.off = m

    def alloc(self, shape, dtype, parts=128):
        n = int(np.prod(shape))
        words = n if dtype == F32 else (n + 1) // 2
        a = self.t[0:parts, self.off:self.off + words]
        self.off += words
        assert self.off <= self.n, f"arena overflow {self.off} > {self.n}"
        if dtype == BF16:
            a = a.bitcast(BF16)
            if 2 * words != n:
                a = a[:, 0:n]
        if len(shape) == 2:
            a = a.rearrange("p (a b) -> p a b", a=shape[0], b=shape[1])
        elif len(shape) == 3:
            a = a.rearrange("p (a b c) -> p a b c", a=shape[0], b=shape[1], c=shape[2])
        return a


def _consts():
    c32 = {}
    cb = {}
    s = np.arange(128)[:, None]
    c = np.arange(128)[None, :]
    g = -1.0 / 16.0
    c32['Uf'] = np.where(s <= c, g, 0.0)
    c32['Ub'] = np.where(s >= c, g, 0.0)
    c32['UfX'] = np.where(s > c, g, 0.0)
    c32['UbX'] = np.where(s < c, g, 0.0)
    bm = np.zeros((128, 256)); mh = np.zeros((128, 4))
    for h in range(4):
        bm[h * 32:(h + 1) * 32, h * 64:(h + 1) * 64] = 1.0
        mh[h * 32:(h + 1) * 32, h] = 32.0 ** -0.5
    c32['bm'] = bm
    c32['maskh'] = mh
    c32['ident'] = np.eye(128)
    t = np.arange(L)
    row = (t // 64).astype(np.float32); col = (t % 64).astype(np.float32)
    inv = (1.0 / (np.float32(10000.0) ** (np.arange(16, dtype=np.float32) / np.float32(16)))).astype(np.float32)
    ang_r = row[:, None] * inv[None, :]
    ang_c = col[:, None] * inv[None, :]
    cos = np.zeros((64, L), np.float32); sin = np.zeros((64, L), np.float32)
    for d in range(64):
        a = ang_r if d < 32 else ang_c
        cos[d] = np.cos(a[:, d % 16]); sin[d] = np.sin(a[:, d % 16])
    rt = np.zeros((64, 64), np.float32)
    for m in range(64):
        if (m % 32) < 16:
            rt[m + 16, m] = -1.0
        else:
            rt[m - 16, m] = 1.0
    cb['ident'] = np.eye(128)
    cb['maskF'] = np.broadcast_to(np.where(s <= c, 1.0, 0.0)[:, None, :], (128, 4, 128)).reshape(128, 512)
    cb['maskB'] = np.broadcast_to(np.where(s >= c, 1.0, 0.0)[:, None, :], (128, 4, 128)).reshape(128, 512)
    rtp = np.zeros((128, 128)); rtp[:64, :64] = rt; rtp[64:, 64:] = rt
    cb['RT'] = rtp
    c32['ones'] = np.ones((128, 128))
    off32 = {}; o = 0
    for k, v in c32.items():
        off32[k] = (o, v.shape[1]); o += v.shape[1]
    a32 = np.concatenate([v for v in c32.values()], axis=1).astype(np.float32)
    offb = {}; o = 0
    for k, v in cb.items():
        offb[k] = (o, v.shape[1]); o += v.shape[1]
    ab = np.concatenate([v for v in cb.values()], axis=1).astype(ml_dtypes.bfloat16)
    cos = np.concatenate([cos, cos], axis=0); sin = np.concatenate([sin, sin], axis=0)
    return a32, off32, ab, offb, cos, sin


def _na_bias(rpb):
    qc = np.arange(64)
    cstart = np.clip(qc - 8, 0, 48)
    kc = np.arange(64)
    valid = (kc[:, None] >= cstart[None, :]) & (kc[:, None] < cstart[None, :] + 16)
    cidx = np.clip(kc[:, None] - qc[None, :] + 15, 0, 30)
    out = np.full((DEPTH, 4, 2, 64, 14, 64), NEG, np.float32)
    for d0i in range(14):
        d0 = d0i - 7
        for rr in range(2):
            dr = d0 + rr
            if dr < -7 or dr > 7:
                continue
            g = rpb[:, :, dr + 7, :][:, :, cidx]
            out[:, :, rr, :, d0i, :] = np.where(valid[None, None], g, np.float32(NEG))
    return out.reshape(DEPTH, 4, 128, 14, 64)


class K:
    def __init__(self, debug=False, layers=(0, 1), stop_after=None):
        self.debug = debug
        self.layers = layers
        self.stop_after = stop_after
        self.nc = bass.Bass("TRN2", target_bir_lowering=False)
        self.es = contextlib.ExitStack()
        self.S = Sched(self.nc, self.es)
        self.A = Arena(self.nc, self.es, 51500)
        self.ps = [self.es.enter_context(self.nc.psum_tensor(f"ps{i}", [128, 512], F32)) for i in range(8)]
        self.din = {}
        self.dsc = {}
        self.uid = 0

    def inp(self, name, shape, dtype=F32):
        self.din[name] = self.nc.dram_tensor(name, list(shape), dtype, kind="ExternalInput").ap()
        return self.din[name]

    def scratch(self, name, shape, dtype, dbg=True):
        kind = "ExternalOutput" if (self.debug and dbg) else "Internal"
        self.dsc[name] = self.nc.dram_tensor(name, list(shape), dtype, kind=kind).ap()
        return self.dsc[name]

    def mm(self, out, lhsT, rhs, start, stop, r, w):
        self.S.op('pe', lambda e: e.matmul(out, lhsT=lhsT, rhs=rhs, start=start, stop=stop), r, w)

    def tr(self, out, in_, ident, r, w):
        self.S.op('pe', lambda e: e.transpose(out=out, in_=in_, identity=ident), r, w)

    def act(self, out, in_, func, r, w, **kw):
        self.S.op('act', lambda e: e.activation(out=out, in_=in_, func=func, **kw), r, w)

    def tt(self, eng, out, in0, in1, op, r, w):
        self.S.op(eng, lambda e: e.tensor_tensor(out=out, in0=in0, in1=in1, op=op), r, w)

    def ts(self, eng, out, in0, s1, s2, op0, op1, r, w, **kw):
        if s2 is None:
            self.S.op(eng, lambda e: e.tensor_scalar(out=out, in0=in0, scalar1=s1, scalar2=None, op0=op0, **kw), r, w)
        else:
            self.S.op(eng, lambda e: e.tensor_scalar(out=out, in0=in0, scalar1=s1, scalar2=s2, op0=op0, op1=op1, **kw), r, w)

    def stt(self, eng, out, in0, scalar, in1, op0, op1, r, w, **kw):
        self.S.op(eng, lambda e: e.scalar_tensor_tensor(out=out, in0=in0, scalar=scalar, in1=in1, op0=op0, op1=op1, **kw), r, w)

    def cp(self, eng, out, in_, r, w):
        if eng == 'act':
            self.S.op('act', lambda e: e.activation(out=out, in_=in_, func=AF.Copy), r, w)
        else:
            self.S.op(eng, lambda e: e.tensor_copy(out=out, in_=in_), r, w)

    def red(self, eng, out, in_, op, r, w):
        self.S.op(eng, lambda e: e.tensor_reduce(out=out, in_=in_, axis=AX.X, op=op), r, w)

    def recip(self, out, in_, r, w):
        self.S.op('dve', lambda e: e.reciprocal(out=out, in_=in_), r, w)

    def memset(self, eng, ap, val, w):
        self.S.op(eng, lambda e: e.memset(ap, val), (), w)

    def dma(self, q, out, in_, r=(), w=(), own=False):
        self.S.dma(q, out, in_, r, w, own=own)
        if q == 'pool' and not own:
            self.npool = getattr(self, 'npool', 0) + 1
            if self.npool % 4 == 0:
                self.issue_cast()

    def issue_cast(self):
        pc = getattr(self, 'pending_casts', [])
        if not pc:
            return
        d_, s_, key = pc.pop(0)
        k = getattr(self, 'ncast', 0)
        self.ncast = k + 1
        self.S.dma('pool', d_, s_, (), [key], own=True)

    def flush_casts(self, prefix):
        while getattr(self, 'pending_casts', []) and self.pending_casts[0][2].startswith(prefix):
            self.issue_cast()

    def rstd(self, ss, n, key):
        self.ts('dve', ss, ss, 1.0 / n, EPS, ALU.mult, ALU.add, [key], [key])
        self.act(ss, ss, AF.Sqrt, [key], [key])
        self.recip(ss, ss, [key], [key])

    def setup(self):
        nc = self.nc
        a32, off32, ab, offb, cos, sin = _consts()
        self.host_consts = dict(c32=a32, cb=ab, cos=cos, sin=sin)
        inp = self.inp
        inp('xin', [T, D]); inp('cc', [128, 8, 2])
        inp('w_mod', [DEPTH, D, 6 * D]); inp('b_mod', [DEPTH, 6 * D])
        inp('g_norm1', [DEPTH, D]); inp('g_norm2', [DEPTH, D]); inp('g_final', [D])
        inp('w_in', [DEPTH, D, INC]); inp('wup', [DEPTH, 2, 33, 128])
        inp('gla_g', [DEPTH, 256]); inp('diff_lambda', [DEPTH, 256]); inp('diff_g', [DEPTH, 128])
        inp('nabias', [DEPTH, 4, 128, 14, 64]); inp('w_out', [DEPTH, D, D])
        inp('w_ffn_gate', [1, D, FD]); inp('w_ffn_up', [1, D, FD]); inp('w_ffn_down', [1, FD, D])
        inp('w_routerT', [NE * D]); inp('w_moe_gate', [1, NE, D, FE]); inp('w_moe_up', [1, NE, D, FE])
        inp('w_moe_down', [1, NE, FE, D])
        inp('c32', list(a32.shape)); inp('cb', list(ab.shape), BF16)
        inp('cos', [128, L]); inp('sin', [128, L])
        self.yout = nc.dram_tensor("y", [L, D], F32, kind="ExternalOutput").ap()
        sc = self.scratch
        sc('xres', [T, D], F32)
        sc('gqT', [128, T], BF16); sc('gkT', [128, T], BF16); sc('gdT', [32, T], F32)
        sc('gktm', [T, 128], BF16); sc('gv', [T, 256], BF16); sc('gg', [T, 256], F32)
        sc('dqT', [8, 64, T], BF16); sc('dkT', [8, 64, T], BF16); sc('dva', [T, 4, 129], BF16)
        sc('nqT', [4, 64, T], BF16); sc('nkT', [4, 64, T], BF16); sc('nva', [T, 4, 65], BF16)
        sc('mix', [T, D], BF16); sc('h2T', [128, 8, T], BF16)
        sc('wfg', [D, FD], BF16, dbg=False); sc('wfu', [D, FD], BF16, dbg=False); sc('wfd', [FD, D], BF16, dbg=False)
        sc('wmg', [NE, D, FE], BF16, dbg=False); sc('wmu', [NE, D, FE], BF16, dbg=False); sc('wmd', [NE, FE, D], BF16, dbg=False)
        A = self.A
        self.c32 = A.alloc([a32.shape[1]], F32)
        self.cb = A.alloc([ab.shape[1]], BF16)
        self.dma('sp', self.c32, self.din['c32'][:, :], w=['c32'])
        self.dma('sp', self.cb, self.din['cb'][:, :], w=['cb'])
        self.k32 = {k: self.c32[:, o:o + n] for k, (o, n) in off32.items()}
        self.kb = {k: self.cb[:, o:o + n] for k, (o, n) in offb.items()}
        self.modL = A.alloc([6 * D], F32); self.modC = A.alloc([6 * D], F32)
        self.gates = A.alloc([32, 8], F32)
        self.S.barrier()
        self.pending_casts = []
        self.castkeys = {}
        if 0 in self.layers and self.stop_after is None:
            for nm, src in (('wfg', 'w_ffn_gate'), ('wfu', 'w_ffn_up'), ('wfd', 'w_ffn_down')):
                s_ = self.din[src][0].rearrange("(p a) f -> p a f", p=128)
                d_ = self.dsc[nm].rearrange("(p a) f -> p a f", p=128)
                na_ = s_.shape[1]
                for a0 in range(0, na_, max(1, na_ // 8)):
                    a1 = min(na_, a0 + max(1, na_ // 8))
                    self.pending_casts.append((d_[:, a0:a1, :], s_[:, a0:a1, :], f'@{nm}_{a0}'))
                    self.castkeys.setdefault('@' + nm, []).append(f'@{nm}_{a0}')
        if 1 in self.layers and self.stop_after is None:
            for e in range(NE):
                for nm, src in (('wmg', 'w_moe_gate'), ('wmu', 'w_moe_up'), ('wmd', 'w_moe_down')):
                    s_ = self.din[src][0, e].rearrange("(p a) f -> p a f", p=128)
                    d_ = self.dsc[nm][e].rearrange("(p a) f -> p a f", p=128)
                    na_ = s_.shape[1]
                    for a0 in range(0, na_, max(1, na_ // 8)):
                        a1 = min(na_, a0 + max(1, na_ // 8))
                        self.pending_casts.append((d_[:, a0:a1, :], s_[:, a0:a1, :], f'@{nm}{e}_{a0}'))
                        self.castkeys.setdefault(f'@{nm}{e}', []).append(f'@{nm}{e}_{a0}')

    def phase0_mod(self, l):
        A = self.A; m0 = A.mark()
        cc = A.alloc([8, 2], F32); sc_ = A.alloc([8, 2], F32)
        rep = A.alloc([16, 128], F32)
        self.dma('sp', cc, self.din['cc'][:, :, :], w=['cc'])
        self.act(sc_, cc, AF.Silu, ['cc'], ['sc'])
        for j in range(8):
            for v in range(2):
                self.ts('dve', rep[:, j * 2 + v, :], self.k32['ones'], sc_[:, j, v:v + 1], None, ALU.mult, None,
                        ['sc', 'c32'], [f'rep{j}{v}'])
        wm = [A.alloc([8, 512], F32) for _ in range(2)]
        bm_ = [A.alloc([512], F32) for _ in range(2)]
        for nb in range(12):
            b = nb % 2
            self.dma('sp', wm[b], self.din['w_mod'][l, :, nb * 512:(nb + 1) * 512].rearrange("(j p) n -> p j n", p=128),
                     w=[f'wm{b}'])
            self.dma('sp', bm_[b], self.din['b_mod'][l, nb * 512:(nb + 1) * 512].partition_broadcast(128), w=[f'bmod{b}'])
            for v, mod in enumerate((self.modL, self.modC)):
                pst = self.ps[v]
                for j in range(8):
                    self.mm(pst[:, :], rep[:, j * 2 + v, :], wm[b][:, j, :], j == 0, j == 7,
                            [f'rep{j}{v}', f'wm{b}'], [f'ps{v}'])
                self.tt('dve', mod[:, nb * 512:(nb + 1) * 512], pst[:, :], bm_[b], ALU.add,
                        [f'ps{v}', f'bmod{b}'], [f'mod{v}'])
        gB = A.alloc([D], F32)
        for gi, gname in enumerate(('g_norm1', 'g_norm2')):
            self.dma('sp', gB, self.din[gname][l].partition_broadcast(128), w=['gB'])
            o = 1024 + gi * 3072
            for v, mod in enumerate((self.modL, self.modC)):
                self.stt('dve', mod[:, o:o + 1024], mod[:, o:o + 1024], 1.0, gB, ALU.add, ALU.mult,
                         [f'mod{v}', 'gB'], [f'mod{v}'])
        self.S.barrier()
        A.reset(m0)

    def norm_mod(self, xt, mod, off, hout, kx, kss, kh, khout, ss, junk, htmp, kjunk='junk'):
        self.act(junk, xt, AF.Square, [kx], [kjunk, kss], accum_out=ss)
        self.rstd(ss, D, kss)
        self.stt('dve', htmp, xt, ss[:, 0:1], mod[:, off + 1024:off + 2048], ALU.mult, ALU.mult, [kx, kss, 'mod'], [kh])
        self.tt('pool', hout, htmp, mod[:, off:off + 1024], ALU.add, [kh, 'mod'], [khout])

    def transpose8(self, src_bf, dst, ksrc, kdst, bank=7):
        pv = self.ps[bank][:, :].bitcast(BF16)
        for j in range(8):
            self.tr(pv[:, j * 128:(j + 1) * 128], src_bf[:, j * 128:(j + 1) * 128], self.kb['ident'],
                    [ksrc, 'cb'], [f'ps{bank}'])
        self.cp('act', dst, pv.rearrange("p (j t) -> p j t", j=8), [f'ps{bank}'], [kdst])

    def phase1_inproj(self, l):
        A = self.A; m0 = A.mark()
        xsrc = self.din['xin'] if l == 0 else self.dsc['xres']
        win = A.alloc([8, INC], BF16)
        self.dma('pool', win, self.din['w_in'][l].rearrange("(j p) n -> p j n", p=128), w=['win'])
        cos = A.alloc([L], F32); sin = A.alloc([L], F32)
        self.dma('sp', cos, self.din['cos'][:, :], w=['cos'])
        self.dma('sp', sin, self.din['sin'][:, :], w=['sin'])
        xt = [A.alloc([D], F32) for _ in range(2)]
        htmp2 = [A.alloc([D], F32) for _ in range(2)]; junk_ = A.alloc([D], F32); junk2 = [junk_, junk_]
        hb = [A.alloc([D], BF16) for _ in range(2)]
        ss = [A.alloc([1], F32) for _ in range(2)]
        hT = [A.alloc([8, 512], BF16) for _ in range(2)]
        stg = [A.alloc([512], BF16) for _ in range(3)]
        stgd = A.alloc([512], F32)
        xb = [A.alloc([512], BF16) for _ in range(2)]
        t1 = [A.alloc([512], F32) for _ in range(2)]
        t2 = [A.alloc([512], F32) for _ in range(2)]
        skv = [A.alloc([384], BF16) for _ in range(2)]
        sgg = [A.alloc([256], F32) for _ in range(2)]
        sdv = [A.alloc([4, 129], BF16) for _ in range(2)]
        snv = [A.alloc([4, 65], BF16) for _ in range(2)]
        for b in range(2):
            self.memset('pool', sdv[b][:, :, 128:129], 1.0, [f'sdv{b}'])
            self.memset('pool', snv[b][:, :, 64:65], 1.0, [f'snv{b}'])
        chunks = [('gqT', None, 0, 128), ('gkT', None, 128, 128), ('gdT', None, 768, 32)]
        chunks += [('dqT', p_, 800 + p_ * 128, 128) for p_ in range(4)]
        chunks += [('dkT', p_, 1312 + p_ * 128, 128) for p_ in range(4)]
        chunks += [('nqT', p_, 2336 + p_ * 128, 128) for p_ in range(2)]
        chunks += [('nkT', p_, 2592 + p_ * 128, 128) for p_ in range(2)]
        nst = 0; nrope = 0; ntm = 0
        for tb in range(9):
            nsub = 4 if tb < 8 else 2
            N = nsub * 128; tok0 = tb * 512; isctx = tb == 8
            mod = self.modC if isctx else self.modL
            hTb = hT[tb % 2]; khT = f'hT{tb % 2}'
            for s in range(nsub):
                t = tb * 4 + s; b = t % 2
                self.dma('sp', xt[b], xsrc[t * 128:(t + 1) * 128, :], w=[f'xt{b}'])
                self.norm_mod(xt[b], mod, 0, hb[b], f'xt{b}', f'ss{b}', f'htmp{b}', f'hb{b}', ss[b], junk2[b], htmp2[b], kjunk='junk')
                self.transpose8(hb[b], hTb[:, :, s * 128:(s + 1) * 128], f'hb{b}', khT)
            for ci, (name, idx, c0, M) in enumerate(chunks):
                bank = ci % 3; pst = self.ps[bank]
                for j in range(8):
                    self.mm(pst[0:M, 0:N], win[:, j, c0:c0 + M], hTb[:, j, 0:N], j == 0, j == 7, ['win', khT], [f'ps{bank}'])
                dst = self.dsc[name]
                if name == 'gdT':
                    self.cp('dve', stgd[0:32, 0:N], pst[0:32, 0:N], [f'ps{bank}'], ['stgd'])
                    self.dma('pool', dst[0:32, tok0:tok0 + N], stgd[0:32, 0:N], r=['stgd'])
                    continue
                dap = dst[:, tok0:tok0 + N] if idx is None else \
                    dst.rearrange("a d t -> (a d) t")[idx * 128:(idx + 1) * 128, tok0:tok0 + N]
                sg = stg[nst % 3]; ksg = f'stg{nst % 3}'; nst += 1
                if name in ('dqT', 'dkT') and not isctx:
                    rb = nrope % 2; nrope += 1
                    self.cp('act', xb[rb][:, 0:N], pst[:, 0:N], [f'ps{bank}'], [f'xb{rb}'])
                    psr = self.ps[3 + rb]
                    self.mm(psr[:, 0:N], self.kb['RT'], xb[rb][:, 0:N], True, True, ['cb', f'xb{rb}'], [f'ps{3 + rb}'])
                    self.tt('dve', t1[rb][:, 0:N], xb[rb][:, 0:N], cos[:, tok0:tok0 + N], ALU.mult, [f'xb{rb}', 'cos'], [f't1{rb}'])
                    self.tt('dve', t2[rb][:, 0:N], psr[:, 0:N], sin[:, tok0:tok0 + N], ALU.mult, [f'ps{3 + rb}', 'sin'], [f't2{rb}'])
                    self.tt('pool', sg[:, 0:N], t1[rb][:, 0:N], t2[rb][:, 0:N], ALU.add, [f't1{rb}', f't2{rb}'], [ksg])
                else:
                    self.cp('act', sg[0:M, 0:N], pst[0:M, 0:N], [f'ps{bank}'], [ksg])
                self.dma('pool', dap, sg[0:M, 0:N], r=[ksg])
            for s in range(nsub):
                t = tb * 4 + s; b = ntm % 2; ntm += 1
                rows = slice(t * 128, (t + 1) * 128)
                lh = lambda j: hTb[:, j, s * 128:(s + 1) * 128]
                pa, pb_, pd, pn = self.ps[5], self.ps[6], self.ps[3 + (s % 2)], self.ps[6]
                for j in range(8):
                    self.mm(pa[:, 0:512], lh(j), win[:, j, 128:640], j == 0, j == 7, ['win', khT], ['ps5'])
                self.cp('dve', skv[b], pa[:, 0:384], ['ps5'], [f'skv{b}'])
                self.cp('dve', sgg[b][:, 0:128], pa[:, 384:512], ['ps5'], [f'sgg{b}'])
                for j in range(8):
                    self.mm(pb_[:, 0:128], lh(j), win[:, j, 640:768], j == 0, j == 7, ['win', khT], ['ps6'])
                self.cp('dve', sgg[b][:, 128:256], pb_[:, 0:128], ['ps6'], [f'sgg{b}'])
                self.dma('pool', self.dsc['gktm'][rows, :], skv[b][:, 0:128], r=[f'skv{b}'])
                self.dma('pool', self.dsc['gv'][rows, :], skv[b][:, 128:384], r=[f'skv{b}'])
                self.dma('pool', self.dsc['gg'][rows, :], sgg[b], r=[f'sgg{b}'])
                kpd = f'ps{3 + (s % 2)}'
                for j in range(8):
                    self.mm(pd[:, 0:512], lh(j), win[:, j, 1824:2336], j == 0, j == 7, ['win', khT], [kpd])
                self.cp('act', sdv[b][:, :, 0:128], pd[:, 0:512].rearrange("p (h j) -> p h j", h=4), [kpd], [f'sdv{b}'])
                self.dma('pool', self.dsc['dva'][rows, :, :], sdv[b], r=[f'sdv{b}'])
                for j in range(8):
                    self.mm(pn[:, 0:256], lh(j), win[:, j, 2848:3104], j == 0, j == 7, ['win', khT], ['ps6'])
                self.cp('dve', snv[b][:, :, 0:64], pn[:, 0:256].rearrange("p (h j) -> p h j", h=4), ['ps6'], [f'snv{b}'])
                self.dma('pool', self.dsc['nva'][rows, :, :], snv[b], r=[f'snv{b}'])
        self.S.barrier()
        A.reset(m0)

    def gla_dir(self, B, t, d, need_att, need_u):
        tok = slice(t * 128, (t + 1) * 128)
        kd = f'd{d}'
        U = self.k32['Uf' if d == 0 else 'Ub']; UX = self.k32['UfX' if d == 0 else 'UbX']
        ps = self.ps
        self.mm(ps[0][:, 0:128], B['gda'][0:33, tok], B['wupt'][0:33, d, :], True, True, ['gda', 'wupt'], ['ps0'])
        self.act(B['e1'], ps[0][:, 0:128], AF.Exp, ['ps0'], ['e1'], scale=-1.0)
        self.act(B['sp'][d], B['e1'], AF.Ln, ['e1'], ['sp' + kd], bias=1.0)
        self.mm(ps[1][:, 0:128], B['sp'][d], U, True, True, ['sp' + kd, 'c32'], ['ps1'])
        self.act(B['eb'][d], ps[1][:, 0:128], AF.Exp, ['ps1'], ['eb' + kd])
        if need_att:
            self.act(B['enb'], ps[1][:, 0:128], AF.Exp, ['ps1'], ['enb'], scale=-1.0)
            self.tt('dve', B['kinvT'], B['gkT'][:, tok], B['enb'], ALU.mult, ['gkT', 'enb'], ['kinvT'])
            for h in range(4):
                self.stt('dve', B['Qblk'][d][:, h, :], B['eb'][d], self.k32['maskh'][:, h:h + 1], B['gqT'][:, tok],
                         ALU.mult, ALU.mult, ['eb' + kd, 'c32', 'gqT'], ['Qblk' + kd])
            self.stt('dve', B['qdec'][d], B['eb'][d], 32.0 ** -0.5, B['gqT'][:, tok], ALU.mult, ALU.mult,
                     ['eb' + kd, 'gqT'], ['qdec' + kd])
            self.mm(ps[2][:, 0:512], B['kinvT'], B['Qblk'][d].rearrange("p h c -> p (h c)"), True, True,
                    ['kinvT', 'Qblk' + kd], ['ps2'])
            self.tt('dve', B['attM'][d].rearrange("p h c -> p (h c)"), ps[2][:, 0:512],
                    self.kb['maskF' if d == 0 else 'maskB'], ALU.mult, ['ps2', 'cb'], ['attM' + kd])
        if need_u:
            self.mm(ps[3][:, 0:128], UX, B['sp'][d], True, True, ['c32', 'sp' + kd], ['ps3'])
            self.act(B['ebd'], ps[3][:, 0:128], AF.Exp, ['ps3'], ['ebd'])
            self.tt('dve', B['kend'], B['gktm'][:, t, :], B['ebd'], ALU.mult, ['gktm', 'ebd'], ['kend'])
            self.mm(ps[4][:, 0:256], B['kend'], B['gv'][:, t, :], True, True, ['kend', 'gv'], ['ps4'])
            self.tt('dve', B['um'], ps[4][:, 0:256], self.k32['bm'], ALU.mult, ['ps4', 'c32'], ['um'])
            dec = B['eb'][d][:, 127:128] if d == 0 else B['eb'][d][:, 0:1]
            self.stt('dve', B['S'][d], B['S'][d], dec, B['um'], ALU.mult, ALU.add, ['S' + kd, 'eb' + kd, 'um'], ['S' + kd])

    def phase2_gla(self, l, last):
        A = self.A; m0 = A.mark(); B = {}
        B['gqT'] = A.alloc([T], BF16); B['gkT'] = A.alloc([T], BF16)
        B['gktm'] = A.alloc([NT, 128], BF16); B['gv'] = A.alloc([NT, 256], BF16)
        B['gda'] = A.alloc([T], F32); B['wupt'] = A.alloc([2, 128], F32)
        gnB = A.alloc([256], F32)
        SprevF = A.alloc([NT, 256], BF16)
        B['S'] = [A.alloc([256], F32) for _ in range(2)]
        Sbbf = [A.alloc([256], BF16) for _ in range(2)]
        B['e1'] = A.alloc([128], F32); B['sp'] = [A.alloc([128], F32) for _ in range(2)]
        B['eb'] = [A.alloc([128], F32) for _ in range(2)]; B['enb'] = A.alloc([128], F32)
        B['kinvT'] = A.alloc([128], BF16); B['Qblk'] = [A.alloc([4, 128], BF16) for _ in range(2)]
        B['qdec'] = [A.alloc([128], BF16) for _ in range(2)]; B['attM'] = [A.alloc([4, 128], BF16) for _ in range(2)]
        B['ebd'] = A.alloc([128], F32); B['kend'] = A.alloc([128], BF16); B['um'] = A.alloc([256], F32)
        osb = A.alloc([4, 64], F32); sq = A.alloc([4, 64], F32); ssq = A.alloc([4], F32); on = A.alloc([4, 64], F32)
        gg = [A.alloc([256], F32) for _ in range(2)]; sg = A.alloc([256], F32)
        stage = [A.alloc([256], BF16) for _ in range(2)]
        d = self.dsc
        self.dma('sp', B['gqT'], d['gqT'][:, :], w=['gqT']); self.dma('sp', B['gkT'], d['gkT'][:, :], w=['gkT'])
        for t0_ in range(0, NT, 8):
            t1_ = min(NT, t0_ + 8)
            self.dma('sp', B['gktm'][:, t0_:t1_, :], d['gktm'][t0_ * 128:t1_ * 128, :].rearrange("(t p) k -> p t k", p=128), w=['gktm'])
            self.dma('sp', B['gv'][:, t0_:t1_, :], d['gv'][t0_ * 128:t1_ * 128, :].rearrange("(t p) k -> p t k", p=128), w=['gv'])
        self.dma('sp', B['gda'][0:32], d['gdT'][:, :], w=['gda'])
        self.memset('pool', B['gda'][32:33], 1.0, ['gda'])
        self.dma('sp', B['wupt'][0:33], self.din['wup'][l].rearrange("d k n -> k d n"), w=['wupt'])
        self.dma('sp', gnB, self.din['gla_g'][l].partition_broadcast(128), w=['gnB'])
        for dd in range(2):
            self.memset('dve', B['S'][dd], 0.0, [f'Sd{dd}'])
        for t in [32, 33] + list(range(32)):
            self.cp('pool', SprevF[:, t, :], B['S'][0], ['Sd0'], [f'SprevF{t}'])
            self.gla_dir(B, t, 0, False, True)
        ps = self.ps
        for it, t in enumerate([33, 32] + list(range(31, -1, -1))):
            need_out = not (last and t >= 32)
            sb_ = Sbbf[it % 2]; ksb = f'Sbbf{it % 2}'
            self.cp('pool', sb_, B['S'][1], ['Sd1'], [ksb])
            if need_out:
                self.gla_dir(B, t, 0, True, False)
            self.gla_dir(B, t, 1, need_out, True)
            if not need_out:
                continue
            self.mm(ps[5][:, 0:256], B['qdec'][0], SprevF[:, t, :], True, False, ['qdecd0', f'SprevF{t}'], ['ps5'])
            self.mm(ps[5][:, 0:256], B['qdec'][1], sb_, False, False, ['qdecd1', ksb], ['ps5'])
            for dd in range(2):
                for h in range(4):
                    self.mm(ps[5][:, h * 64:(h + 1) * 64], B['attM'][dd][:, h, :], B['gv'][:, t, h * 64:(h + 1) * 64],
                            False, dd == 1 and h == 3, [f'attMd{dd}', 'gv'], ['ps5'])
            b = it % 2
            self.dma('sp', gg[b], d['gg'][t * 128:(t + 1) * 128, :], w=[f'gg{b}'])
            self.cp('act', osb.rearrange("p h c -> p (h c)"), ps[5][:, 0:256], ['ps5'], ['osb'])
            self.tt('dve', sq, osb, osb, ALU.mult, ['osb'], ['sq'])
            self.red('dve', ssq, sq, ALU.add, ['sq'], ['ssq'])
            self.rstd(ssq, 64, 'ssq')
            for h in range(4):
                self.stt('dve', on[:, h, :], osb[:, h, :], ssq[:, h:h + 1], gnB[:, h * 64:(h + 1) * 64], ALU.mult, ALU.mult,
                         ['osb', 'ssq', 'gnB'], ['on'])
            self.act(sg, gg[b], AF.Silu, [f'gg{b}'], ['sg'])
            self.tt('pool', stage[b], on.rearrange("p h c -> p (h c)"), sg, ALU.mult, ['on', 'sg'], [f'stage{b}'])
            self.dma('pool', d['mix'][t * 128:(t + 1) * 128, 0:256], stage[b], r=[f'stage{b}'])
        self.S.barrier()
        A.reset(m0)

    def phase3_diff(self, l, last):
        A = self.A; m0 = A.mark(); d = self.dsc; ps = self.ps
        lam_init = 0.8 - 0.6 * math.exp(-0.3 * l)
        lam4 = A.alloc([4, 64], F32); gdB = A.alloc([128], F32)
        self.dma('sp', lam4, self.din['diff_lambda'][l].partition_broadcast(128), w=['lam4'])
        self.dma('sp', gdB, self.din['diff_g'][l].partition_broadcast(128), w=['gdB'])
        lp = A.alloc([2, 64], F32); ls = A.alloc([2], F32); neglam = A.alloc([1], F32)
        self.tt('dve', lp, lam4[:, 0:4:2, :], lam4[:, 1:4:2, :], ALU.mult, ['lam4'], ['lp'])
        self.red('dve', ls, lp, ALU.add, ['lp'], ['ls'])
        self.act(ls, ls, AF.Exp, ['ls'], ['ls'])
        self.tt('dve', neglam, ls[:, 1:2], ls[:, 0:1], ALU.subtract, ['ls'], ['neglam'])
        self.ts('dve', neglam, neglam, -lam_init, None, ALU.add, None, ['neglam'], ['neglam'])
        QT = [A.alloc([T], BF16) for _ in range(2)]; KT = [A.alloc([T], BF16) for _ in range(2)]
        VA = [A.alloc([NT, 129], BF16) for _ in range(2)]
        PT = [A.alloc([512], BF16) for _ in range(4)]
        den = A.alloc([2], F32); t0 = A.alloc([128], F32); a_ = A.alloc([128], F32); junk = A.alloc([128], F32)
        ssd = A.alloc([1], F32); stage = [A.alloc([4, 128], BF16) for _ in range(2)]
        zl = A.alloc([128], BF16); zr = A.alloc([512], BF16)
        self.memset('pool', zl, 0.0, ['zl']); self.memset('pool', zr, 0.0, ['zr'])
        nq = 0; npt = 0
        for h in range(4):
            b = h % 2
            self.dma('sp', QT[b], d['dqT'][2 * h:2 * h + 2].rearrange("m d t -> (m d) t"), w=[f'QT{b}'])
            self.dma('sp', KT[b], d['dkT'][2 * h:2 * h + 2].rearrange("m d t -> (m d) t"), w=[f'KT{b}'])
            for t0_ in range(0, NT, 8):
                t1_ = min(NT, t0_ + 8)
                self.dma('sp', VA[b][:, t0_:t1_, :], d['dva'][t0_ * 128:t1_ * 128, h, :].rearrange("(t p) j -> p t j", p=128), w=[f'VA{b}'])
            qblocks = [(qb * 512, 512, list(range(34))) for qb in range(8)]
            if not last:
                qblocks.append((4096, 256, [32, 33]))
            for (q0, N, kcs) in qblocks:
                nsub = N // 128
                for bo in sorted(set(4 + 2 * m + s_ // 2 for m in range(2) for s_ in range(nsub))):
                    self.mm(ps[bo][:, 0:512], zl, zr, True, False, ['zl', 'zr'], [f'ps{bo}'])
                steps = [(ki, kc, m) for ki, kc in enumerate(kcs) for m in range(2)]

                def s_step(j, npt_):
                    ki, kc, m = steps[j]
                    bank = j % 4
                    self.mm(ps[bank][:, 0:N], KT[b][64 * m:64 * m + 64, kc * 128:(kc + 1) * 128], QT[b][64 * m:64 * m + 64, q0:q0 + N],
                            True, True, [f'KT{b}', f'QT{b}'], [f'ps{bank}'])
                    pt = PT[npt_ % 4]; kpt = f'PT{npt_ % 4}'
                    self.act(pt[:, 0:N], ps[bank][:, 0:N], AF.Exp, [f'ps{bank}'], [kpt], scale=0.125)
                    return pt, kpt

                def pv_step(j, pt, kpt):
                    ki, kc, m = steps[j]
                    for s in range(nsub):
                        bo = 4 + 2 * m + s // 2; c0 = (s % 2) * 256
                        self.mm(ps[bo][:, c0:c0 + 129], pt[:, s * 128:(s + 1) * 128], VA[b][:, kc, :],
                                False, ki == len(kcs) - 1 and s % 2 == 1, [kpt, f'VA{b}'], [f'ps{bo}'])

                prev = None
                for j in range(0, len(steps), 2):
                    cur0 = s_step(j, npt); npt += 1
                    cur1 = s_step(j + 1, npt); npt += 1
                    if prev is not None:
                        pv_step(*prev[0]); pv_step(*prev[1])
                    prev = ((j,) + cur0, (j + 1,) + cur1)
                pv_step(*prev[0]); pv_step(*prev[1])
                st = stage[nq % 2]; kst = f'dstage{nq % 2}'; nq += 1
                for s in range(nsub):
                    c0 = (s % 2) * 256
                    o0 = ps[4 + s // 2][:, c0:c0 + 129]; o1 = ps[6 + s // 2][:, c0:c0 + 129]
                    k0 = f'ps{4 + s // 2}'; k1 = f'ps{6 + s // 2}'
                    self.cp('dve', den[:, 0:1], o0[:, 128:129], [k0], ['den'])
                    self.cp('dve', den[:, 1:2], o1[:, 128:129], [k1], ['den'])
                    self.recip(den, den, ['den'], ['den'])
                    self.tt('dve', den[:, 1:2], den[:, 1:2], neglam, ALU.mult, ['den', 'neglam'], ['den'])
                    self.ts('dve', t0, o0[:, 0:128], den[:, 0:1], None, ALU.mult, None, [k0, 'den'], ['t0'])
                    self.stt('dve', a_, o1[:, 0:128], den[:, 1:2], t0, ALU.mult, ALU.add, [k1, 'den', 't0'], ['a_'])
                    self.memset('dve', ssd, 0.0, ['ssd'])
                    self.stt('dve', junk, a_, 1.0, a_, ALU.mult, ALU.mult, ['a_'], ['djunk', 'ssd'], accum_out=ssd)
                    self.ts('dve', ssd, ssd, 1.0 / 128, EPS, ALU.mult, ALU.add, ['ssd'], ['ssd'])
                    self.act(ssd, ssd, AF.Ln, ['ssd'], ['ssd'])
                    self.act(ssd, ssd, AF.Exp, ['ssd'], ['ssd'], scale=-0.5)
                    self.ts('dve', ssd, ssd, 1.0 - lam_init, None, ALU.mult, None, ['ssd'], ['ssd'])
                    self.stt('dve', st[:, s, :], a_, ssd[:, 0:1], gdB, ALU.mult, ALU.mult, ['a_', 'ssd', 'gdB'], [kst])
                self.dma('pool', d['mix'][q0:q0 + N, 256 + h * 128:256 + (h + 1) * 128].rearrange("(s p) j -> p s j", p=128),
                         st[:, 0:nsub, :], r=[kst])
        self.S.barrier()
        A.reset(m0)

    def phase4_na(self, l, last):
        A = self.A; m0 = A.mark(); d = self.dsc; ps = self.ps
        QT = [A.alloc([T], BF16) for _ in range(2)]; KT = [A.alloc([T], BF16) for _ in range(2)]
        VA = [A.alloc([NT, 65], BF16) for _ in range(2)]; VB = [A.alloc([31, 65], BF16) for _ in range(2)]
        bias = [A.alloc([14, 64], F32) for _ in range(2)]
        tmp = [A.alloc([4, 64], F32) for _ in range(2)]; P = [A.alloc([6, 64], BF16) for _ in range(3)]
        den = A.alloc([4], F32); stage = [A.alloc([4, 64], BF16) for _ in range(2)]
        P2 = [A.alloc([256], BF16) for _ in range(2)]; st2 = [A.alloc([64], BF16) for _ in range(2)]
        nonlocal_ng = [0]
        for h in range(4):
            b = h % 2
            self.dma('sp', QT[b][0:64], d['nqT'][h], w=[f'nQT{b}'])
            self.dma('sp', KT[b][0:64], d['nkT'][h], w=[f'nKT{b}'])
            for t0_ in range(0, NT, 8):
                t1_ = min(NT, t0_ + 8)
                self.dma('sp', VA[b][:, t0_:t1_, :], d['nva'][t0_ * 128:t1_ * 128, h, :].rearrange("(t p) j -> p t j", p=128), w=[f'nVA{b}'])
            for t0_ in range(0, 31, 8):
                t1_ = min(31, t0_ + 8)
                self.dma('sp', VB[b][:, t0_:t1_, :], d['nva'][64 + t0_ * 128:64 + t1_ * 128, h, :].rearrange("(t p) j -> p t j", p=128), w=[f'nVB{b}'])
            self.dma('sp', bias[b], self.din['nabias'][l, h], w=[f'nbias{b}'])
            rd = [f'nKT{b}', f'nQT{b}']
            def na_s(r):
                rs = min(max(r - 4, 0), 56); d0i = rs - r + 7
                bs = r % 3; kbs = f'ps{bs}'
                psS = ps[bs][:, 0:384].rearrange("p (i q) -> p i q", i=6)
                qs = QT[b][0:64, r * 64:(r + 1) * 64]
                for i in range(6):
                    o_i = (rs + 2 * i) * 64 if i < 4 else 4096 + (i - 4) * 128
                    self.mm(psS[:, i, :], KT[b][0:64, o_i:o_i + 128], qs, True, True, rd, [kbs])
                tb_ = tmp[r % 2]; pb = P[r % 3]
                self.stt('dve', tb_, psS[:, 0:4, :], 0.125, bias[b][:, d0i:d0i + 7:2, :], ALU.mult, ALU.add,
                         [kbs, f'nbias{b}'], [f'ntmp{r % 2}'])
                self.act(pb[:, 0:4, :], tb_, AF.Exp, [f'ntmp{r % 2}'], [f'nP{r % 3}'])
                self.act(pb[:, 4:6, :], psS[:, 4:6, :], AF.Exp, [kbs], [f'nP{r % 3}'], scale=0.125)

            def na_pv(r):
                rg = r // 4; r4 = r % 4
                bo = 4 + rg % 2; kbo = f'ps{bo}'
                rs = min(max(r - 4, 0), 56)
                pb = P[r % 3]
                for i in range(6):
                    if i < 4:
                        ro = rs + 2 * i
                        V = VA[b][:, ro // 2, :] if ro % 2 == 0 else VB[b][:, (ro - 1) // 2, :]
                    else:
                        V = VA[b][:, 32 + (i - 4), :]
                    self.mm(ps[bo][0:64, r4 * 65:(r4 + 1) * 65], pb[:, i, :], V, i == 0, i == 5,
                            [f'nP{r % 3}', f'nVA{b}', f'nVB{b}'], [kbo])
                if r4 == 3:
                    nonlocal_ng[0] += 1
                    pso = ps[bo][0:64, 0:260].rearrange("p (r j) -> p r j", r=4)
                    st = stage[nonlocal_ng[0] % 2]; kst = f'nstage{nonlocal_ng[0] % 2}'
                    self.cp('dve', den[0:64], pso[:, :, 64], [kbo], ['nden'])
                    self.recip(den[0:64], den[0:64], ['nden'], ['nden'])
                    for q4 in range(4):
                        self.ts('dve', st[0:64, q4, :], pso[:, q4, 0:64], den[0:64, q4:q4 + 1], None, ALU.mult, None,
                                [kbo, 'nden'], [kst])
                    self.dma('pool', d['mix'][rg * 256:(rg + 1) * 256, 768 + h * 64:768 + (h + 1) * 64].rearrange("(r p) j -> p r j", p=64),
                             st[0:64], r=[kst])

            for r in range(64):
                na_s(r)
                if r > 0:
                    na_pv(r - 1)
            na_pv(63)
            if not last:
                qc = QT[b][0:64, 4096:4352]
                for i in range(2):
                    self.mm(ps[i][:, 0:256], KT[b][0:64, 4096 + i * 128:4096 + (i + 1) * 128], qc, True, True, rd, [f'ps{i}'])
                    self.act(P2[i], ps[i][:, 0:256], AF.Exp, [f'ps{i}'], [f'nP2{i}'], scale=0.125)
                for qs_ in range(2):
                    bo = 4 + qs_; kbo = f'ps{bo}'
                    for i in range(2):
                        self.mm(ps[bo][:, 0:65], P2[i][:, qs_ * 128:(qs_ + 1) * 128], VA[b][:, 32 + i, :], i == 0, i == 1,
                                [f'nP2{i}', f'nVA{b}'], [kbo])
                    self.cp('dve', den[:, 0:1], ps[bo][:, 64:65], [kbo], ['nden'])
                    self.recip(den[:, 0:1], den[:, 0:1], ['nden'], ['nden'])
                    self.ts('dve', st2[qs_], ps[bo][:, 0:64], den[:, 0:1], None, ALU.mult, None, [kbo, 'nden'], [f'nst2{qs_}'])
                    self.dma('pool', d['mix'][4096 + qs_ * 128:4096 + (qs_ + 1) * 128, 768 + h * 64:768 + (h + 1) * 64],
                             st2[qs_], r=[f'nst2{qs_}'])
        self.S.barrier()
        A.reset(m0)

    def phase5_outproj(self, l, last):
        A = self.A; m0 = A.mark(); d = self.dsc; ps = self.ps
        moe = (l % 2 == 1)
        xsrc = self.din['xin'] if l == 0 else d['xres']
        wout = A.alloc([8, D], BF16)
        self.dma('pool', wout, self.din['w_out'][l].rearrange("(j p) n -> p j n", p=128), w=['wout'])
        if moe:
            wrB = A.alloc([8, D], F32)
            self.dma('sp', wrB.rearrange("p e n -> p (e n)"), self.din['w_routerT'].partition_broadcast(128), w=['wrB'])
            junkr = A.alloc([D], F32); lg = A.alloc([8], F32); l2 = A.alloc([8], F32)
            eq1 = A.alloc([8], F32); eq2 = A.alloc([8], F32); sm = A.alloc([4], F32)
        mixt = [A.alloc([D], BF16) for _ in range(2)]; mixT = [A.alloc([8, 128], BF16) for _ in range(2)]
        xt = [A.alloc([D], F32) for _ in range(2)]; tmp2 = [A.alloc([D], F32) for _ in range(2)]; xn = [A.alloc([D], F32) for _ in range(2)]
        junk2 = [A.alloc([D], F32) for _ in range(2)]; ss = [A.alloc([1], F32) for _ in range(2)]; htmp2 = [A.alloc([D], F32) for _ in range(2)]; h2f2 = [A.alloc([D], F32) for _ in range(2)]
        h2b = [A.alloc([D], BF16) for _ in range(2)]; h2Ts = [A.alloc([8, 128], BF16) for _ in range(2)]
        ntiles = 32 if last else 34
        for t in range(ntiles):
            b = t % 2; rows = slice(t * 128, (t + 1) * 128); isctx = t >= 32
            mod = self.modC if isctx else self.modL
            tmp = tmp2[b]; junk = junk2[b]; htmp = htmp2[b]; h2f = h2f2[b]
            self.dma('sp', mixt[b], d['mix'][rows, :], w=[f'mixt{b}'])
            self.dma('sp', xt[b], xsrc[rows, :], w=[f'oxt{b}'])
            self.transpose8(mixt[b], mixT[b], f'mixt{b}', f'mixT{b}', bank=7)
            for half in range(2):
                for j in range(8):
                    self.mm(ps[half][:, 0:512], mixT[b][:, j, :], wout[:, j, half * 512:(half + 1) * 512], j == 0, j == 7,
                            [f'mixT{b}', 'wout'], [f'ps{half}'])
                self.tt('dve', tmp[:, half * 512:(half + 1) * 512], ps[half][:, 0:512],
                        mod[:, 2048 + half * 512:2048 + (half + 1) * 512], ALU.mult, [f'ps{half}'], [f'otmp{b}'])
            self.tt('pool', xn[b], xt[b], tmp, ALU.add, [f'oxt{b}', f'otmp{b}'], [f'xn{b}'])
            self.dma('pool', d['xres'][rows, :], xn[b], r=[f'xn{b}'])
            self.norm_mod(xn[b], mod, 3072, h2f, f'xn{b}', f'oss{b}', f'ohtmp{b}', f'h2f{b}', ss[b], junk, htmp, kjunk=f'ojunk{b}')
            self.cp('act', h2b[b], h2f, [f'h2f{b}'], [f'h2b{b}'])
            self.transpose8(h2b[b], h2Ts[b], f'h2b{b}', f'h2Ts{b}', bank=6)
            self.dma('pool', d['h2T'][:, :, rows], h2Ts[b], r=[f'h2Ts{b}'])
            if moe and not isctx:
                self.memset('dve', lg, 0.0, ['lg'])
                for e in range(NE):
                    self.stt('dve', junkr, h2f, 1.0, wrB[:, e, :], ALU.mult, ALU.mult, [f'h2f{b}', 'wrB'], ['junkr', 'lg'],
                             accum_out=lg[:, e:e + 1])
                m1 = sm[:, 0:1]; m2 = sm[:, 1:2]; ee = sm[:, 2:3]; w1 = sm[:, 3:4]
                self.red('dve', m1, lg, ALU.max, ['lg'], ['sm'])
                self.ts('dve', eq1, lg, m1, None, ALU.is_equal, None, ['lg', 'sm'], ['eq1'])
                self.stt('dve', l2, eq1, -1e30, lg, ALU.mult, ALU.add, ['eq1', 'lg'], ['l2'])
                self.red('dve', m2, l2, ALU.max, ['l2'], ['sm'])
                self.ts('dve', eq2, l2, m2, None, ALU.is_equal, None, ['l2', 'sm'], ['eq2'])
                self.tt('dve', ee, m2, m1, ALU.subtract, ['sm'], ['sm'])
                self.act(ee, ee, AF.Exp, ['sm'], ['sm'])
                self.ts('dve', w1, ee, 1.0, None, ALU.add, None, ['sm'], ['sm'])
                self.recip(w1, w1, ['sm'], ['sm'])
                self.tt('dve', ee, ee, w1, ALU.mult, ['sm'], ['sm'])
                self.ts('dve', self.gates[:, t, :], eq1, w1, None, ALU.mult, None, ['eq1', 'sm'], ['@gates'])
                self.stt('dve', self.gates[:, t, :], eq2, ee, self.gates[:, t, :], ALU.mult, ALU.add, ['eq2', 'sm', '@gates'], ['@gates'])
        self.S.barrier()
        A.reset(m0)

    def phase6_ffn(self, l, last):
        A = self.A; m0 = A.mark(); d = self.dsc; ps = self.ps
        moe = (l % 2 == 1)
        nblocks = 8 if last else 9
        if moe:
            nexp = NE; F = FE
            wg = lambda e: d['wmg'][e]; wu = lambda e: d['wmu'][e]; wd = lambda e: d['wmd'][e]
            kg = lambda e: f'@wmg{e}'; ku = lambda e: f'@wmu{e}'; kd = lambda e: f'@wmd{e}'
        else:
            nexp = 1; F = FD
            wg = lambda e: d['wfg']; wu = lambda e: d['wfu']; wd = lambda e: d['wfd']
            kg = lambda e: '@wfg'; ku = lambda e: '@wfu'; kd = lambda e: '@wfd'
        nfc = F // 128
        groups = [(c0, min(4, nfc - c0)) for c0 in range(0, nfc, 4)]
        h2T = [A.alloc([8, 512], BF16) for _ in range(2)]
        Wg = [A.alloc([8, 512], BF16) for _ in range(3)]; Wu = [A.alloc([8, 512], BF16) for _ in range(3)]
        Wd = [A.alloc([4, D], BF16) for _ in range(3)]
        sgl = [A.alloc([512], F32) for _ in range(2)]; actT = [A.alloc([4, 512], BF16) for _ in range(2)]
        yacc = A.alloc([4, D], F32)
        xt = [A.alloc([D], F32) for _ in range(2)]; tmp = A.alloc([D], F32); xn_ = A.alloc([D], F32); xn = [xn_, xn_]
        junk = A.alloc([D], F32); ss = A.alloc([1], F32); outs_ = A.alloc([D], F32); outs = [outs_, outs_]
        if last:
            gfB = A.alloc([D], F32)
            self.dma('sp', gfB, self.din['g_final'].partition_broadcast(128), w=['gfB'])
        nw = 0; na = 0; nt = 0
        for tb in range(nblocks):
            nsub = 4 if tb < 8 else 2
            N = nsub * 128; tok0 = tb * 512; isctx = tb == 8
            mod = self.modC if isctx else self.modL
            hb = h2T[tb % 2]; khb = f'fh2T{tb % 2}'
            self.dma('sp', hb[:, :, 0:N], d['h2T'][:, :, tok0:tok0 + N], w=[khb])
            def gu(e, c0, nf, nw_, na_):
                wb = nw_ % 3
                fs = slice(c0 * 128, (c0 + nf) * 128)
                self.dma('sp', Wg[wb][:, :, 0:nf * 128], wg(e)[:, fs].rearrange("(j p) f -> p j f", p=128), r=self.castkeys[kg(e)], w=[f'Wg{wb}'])
                self.dma('sp', Wu[wb][:, :, 0:nf * 128], wu(e)[:, fs].rearrange("(j p) f -> p j f", p=128), r=self.castkeys[ku(e)], w=[f'Wu{wb}'])
                self.dma('sp', Wd[wb][:, 0:nf, :], wd(e)[fs, :].rearrange("(c p) n -> p c n", p=128), r=self.castkeys[kd(e)], w=[f'Wd{wb}'])
                at = actT[na_ % 2]; kat = f'actT{na_ % 2}'
                for c in range(nf):
                    bg = c % 2; bu = 2 + c % 2
                    for j in range(8):
                        self.mm(ps[bg][:, 0:N], Wg[wb][:, j, c * 128:(c + 1) * 128], hb[:, j, 0:N], j == 0, j == 7,
                                [f'Wg{wb}', khb], [f'ps{bg}'])
                    for j in range(8):
                        self.mm(ps[bu][:, 0:N], Wu[wb][:, j, c * 128:(c + 1) * 128], hb[:, j, 0:N], j == 0, j == 7,
                                [f'Wu{wb}', khb], [f'ps{bu}'])
                    self.act(sgl[c % 2][:, 0:N], ps[bg][:, 0:N], AF.Silu, [f'ps{bg}'], [f'sgl{c % 2}'])
                    self.tt('dve', at[:, c, 0:N], sgl[c % 2][:, 0:N], ps[bu][:, 0:N], ALU.mult, [f'sgl{c % 2}', f'ps{bu}'], [kat])
                return (e, nf, wb, at, kat)

            def down(e, nf, wb, at, kat, first):
                for s in range(nsub):
                    for half in range(2):
                        by = 4 + (s * 2 + half) % 4
                        for c in range(nf):
                            self.mm(ps[by][:, 0:512], at[:, c, s * 128:(s + 1) * 128], Wd[wb][:, c, half * 512:(half + 1) * 512],
                                    c == 0, c == nf - 1, [kat, f'Wd{wb}'], [f'ps{by}'])
                        ya = yacc[:, s, half * 512:(half + 1) * 512]
                        if moe:
                            g_ = self.gates[:, tb * 4 + s, e:e + 1]
                            if first:
                                self.ts('dve', ya, ps[by][:, 0:512], g_, None, ALU.mult, None, [f'ps{by}', '@gates'], ['yacc'])
                            else:
                                self.stt('dve', ya, ps[by][:, 0:512], g_, ya, ALU.mult, ALU.add, [f'ps{by}', '@gates', 'yacc'], ['yacc'])
                        else:
                            if first:
                                self.cp('dve', ya, ps[by][:, 0:512], [f'ps{by}'], ['yacc'])
                            else:
                                self.tt('dve', ya, ps[by][:, 0:512], ya, ALU.add, [f'ps{by}', 'yacc'], ['yacc'])

            items = [(e, c0, nf) for e in range(nexp) for (c0, nf) in groups]
            prev = None; first = True
            for (e, c0, nf) in items:
                cur = gu(e, c0, nf, nw, na); nw += 1; na += 1
                if prev is not None:
                    down(*prev, first); first = False
                prev = cur
            down(*prev, first)
            for s in range(nsub):
                t = tb * 4 + s; b = nt % 2; nt += 1
                rows = slice(t * 128, (t + 1) * 128)
                self.dma('sp', xt[b], d['xres'][rows, :], w=[f'fxt{b}'])
                self.tt('dve', tmp, yacc[:, s, :], mod[:, 5120:6144], ALU.mult, ['yacc'], ['ftmp'])
                self.tt('pool', xn[b], xt[b], tmp, ALU.add, [f'fxt{b}', 'ftmp'], ['fxn'])
                if last:
                    self.act(junk, xn[b], AF.Square, ['fxn'], ['fjunk', 'fss'], accum_out=ss)
                    self.rstd(ss, D, 'fss')
                    self.stt('dve', outs[b], xn[b], ss[:, 0:1], gfB, ALU.mult, ALU.mult, ['fxn', 'fss', 'gfB'], ['outs'])
                    self.dma('pool', self.yout[rows, :], outs[b], r=['outs'])
                else:
                    self.dma('pool', d['xres'][rows, :], xn[b], r=['fxn'])
        self.S.barrier()
        A.reset(m0)

    def build(self):
        self.setup()
        for l in self.layers:
            last = (l == DEPTH - 1)
            self.phase0_mod(l)
            self.phase1_inproj(l)
            if self.stop_after == 'inproj':
                break
            self.phase2_gla(l, last)
            if self.stop_after == 'gla':
                break
            self.phase3_diff(l, last)
            if self.stop_after == 'diff':
                break
            self.phase4_na(l, last)
            if self.stop_after == 'na':
                break
            self.phase5_outproj(l, last)
            if self.stop_after == 'outproj':
                break
            self.flush_casts('@wf' if l == 0 else '@wm')
            self.phase6_ffn(l, last)
        self.S.emit()
        return self.nc


def _host_inputs(inputs):
    f = lambda k: np.asarray(inputs[k], dtype=np.float32)
    a32, off32, ab, offb, cos, sin = _consts()
    wup = np.zeros((DEPTH, 2, 33, 128), np.float32)
    wdu = f('gla_w_dec_up'); bde = f('gla_b_dec')
    wup[:, 0, 0:16] = wdu[:, 0]; wup[:, 1, 16:32] = wdu[:, 1]
    wup[:, 0, 32] = bde[:, 0]; wup[:, 1, 32] = bde[:, 1]
    shared = {
        'w_mod': f('w_mod'), 'b_mod': f('b_mod'), 'g_norm1': f('g_norm1'), 'g_norm2': f('g_norm2'),
        'g_final': f('g_final'), 'w_in': f('w_in'), 'wup': wup,
        'gla_g': np.ascontiguousarray(np.tile(f('gla_g_norm'), (1, 4))),
        'diff_lambda': np.ascontiguousarray(f('diff_lambda').reshape(DEPTH, 256)),
        'diff_g': f('diff_g_norm'), 'nabias': _na_bias(f('na_rpb')), 'w_out': f('w_out'),
        'w_ffn_gate': f('w_ffn_gate'), 'w_ffn_up': f('w_ffn_up'), 'w_ffn_down': f('w_ffn_down'),
        'w_routerT': np.ascontiguousarray(f('w_router')[0].T).reshape(-1),
        'w_moe_gate': f('w_moe_gate'), 'w_moe_up': f('w_moe_up'), 'w_moe_down': f('w_moe_down'),
        'c32': a32, 'cb': ab, 'cos': cos, 'sin': sin,
    }
    x = f('x'); ctx = f('ctx'); c = f('c'); cctx = f('c_ctx')
    maps = []
    for b in range(8):
        m = dict(shared)
        m['xin'] = np.ascontiguousarray(np.concatenate([x[b], ctx[b]], axis=0))
        cc = np.stack([c[b], cctx], axis=-1)
        m['cc'] = np.ascontiguousarray(cc.reshape(8, 128, 2).transpose(1, 0, 2))
        maps.append(m)
    return maps


_NC_CACHE = {}


def kernel(**inputs):
    if 'nc' not in _NC_CACHE:
        _NC_CACHE['nc'] = K().build()
    nc = _NC_CACHE['nc']
    maps = _host_inputs(inputs)
    res = run_bass_kernel_spmd(nc, maps, core_ids=list(range(8)))
    return np.stack([np.asarray(r['y'], dtype=np.float32) for r in res.results], axis=0)
```
